# Optimizing a Trainium2 kernel written in Bass

```python
import jax, jax.numpy as jnp
from jax import lax
import numpy as np

D_MODEL = 2048
BATCH = 4
SEQ = 2048
DEPTH = 1

N_META = 16
HEAD_DIM = 64
D_ATTN = D_MODEL // 2
ATTN_HEADS = D_ATTN // HEAD_DIM
ATTN_KV_HEADS = ATTN_HEADS // 4
ATTN_GROUP = ATTN_HEADS // ATTN_KV_HEADS
D_KV = ATTN_KV_HEADS * HEAD_DIM
WINDOW = 128
ATTN_BLOCK = 128
REL_BUCKETS = 32
REL_MAX_DIST = 128
SSM_D_INNER = D_MODEL - D_ATTN
SSM_HEAD_DIM = 64
SSM_HEADS = SSM_D_INNER // SSM_HEAD_DIM
SSM_GROUPS = 2
SSM_HPG = SSM_HEADS // SSM_GROUPS
SSM_STATE = 128
CONV_WIDTH = 4
SSD_CHUNK = 128
D_XBC = SSM_D_INNER + 2 * SSM_GROUPS * SSM_STATE
D_IN_PROJ = D_ATTN + 2 * D_KV + SSM_D_INNER + D_XBC + SSM_HEADS
D_MIX = D_ATTN + SSM_D_INNER
IN_SPLITS = (D_ATTN, D_ATTN + D_KV, D_ATTN + 2 * D_KV, D_ATTN + 2 * D_KV + SSM_D_INNER,
             D_ATTN + 2 * D_KV + SSM_D_INNER + D_XBC)
PEER_HEADS = 8
PEER_TOPK = 16
N_KEYS = 128
N_EXPERTS = N_KEYS * N_KEYS
PEER_HALF = 128
PEER_KEY_DIM = 2 * PEER_HALF
PEER_BLOCK = 128
EPS = 1e-6
NEG = -1e30

kernel_name = "hymba_swa_ssd_peer_block"


def rmsnorm(x, w):
    xf = x.astype(jnp.float32)
    y = xf * lax.rsqrt(jnp.mean(xf * xf, axis=-1, keepdims=True) + EPS)
    return (y * w.astype(jnp.float32)).astype(x.dtype)


def t5_bucket(dist):
    n = np.maximum(dist, 0)
    max_exact = REL_BUCKETS // 2
    large = max_exact + (np.log(np.maximum(n, 1) / max_exact) / np.log(REL_MAX_DIST / max_exact)
                         * (REL_BUCKETS - max_exact)).astype(np.int32)
    large = np.minimum(large, REL_BUCKETS - 1)
    return np.where(n < max_exact, n, large).astype(np.int32)


def band_structure(nb):
    blk = np.arange(nb)[:, None]
    kj = np.arange(ATTN_BLOCK)[None, :]
    qpos = N_META + blk[:, :, None] * ATTN_BLOCK + np.arange(ATTN_BLOCK)[None, :, None]
    kpos = np.concatenate([np.broadcast_to(np.arange(N_META)[None, :], (nb, N_META)),
                           N_META + (blk - 1) * ATTN_BLOCK + kj,
                           N_META + blk * ATTN_BLOCK + kj], axis=1)
    exists = np.concatenate([np.ones((nb, N_META), bool),
                             np.broadcast_to(blk > 0, (nb, ATTN_BLOCK)),
                             np.ones((nb, ATTN_BLOCK), bool)], axis=1)
    is_meta = np.concatenate([np.ones((nb, N_META), bool),
                              np.zeros((nb, 2 * ATTN_BLOCK), bool)], axis=1)
    dist = qpos - kpos[:, None, :]
    valid = exists[:, None, :] & (dist >= 0) & ((dist < WINDOW) | is_meta[:, None, :])
    return t5_bucket(dist), valid


def sink_softmax(logits, sink):
    s = sink.astype(jnp.float32)[..., None, None]
    m = jnp.maximum(jnp.max(logits, axis=-1, keepdims=True), s)
    e = jnp.exp(logits - m)
    return e / (jnp.sum(e, axis=-1, keepdims=True) + jnp.exp(s - m))


def swa_sink_attention(q, k, v, sinks, rel_bias):
    b, L, _ = q.shape
    S = L - N_META
    nb = S // ATTN_BLOCK
    q = q.reshape(b, L, ATTN_KV_HEADS, ATTN_GROUP, HEAD_DIM) * (HEAD_DIM ** -0.5)
    k = k.reshape(b, L, ATTN_KV_HEADS, HEAD_DIM)
    v = v.reshape(b, L, ATTN_KV_HEADS, HEAD_DIM)
    bias_tab = rel_bias.astype(jnp.float32).reshape(REL_BUCKETS, ATTN_KV_HEADS, ATTN_GROUP)
    sink = sinks.reshape(ATTN_KV_HEADS, ATTN_GROUP)
    qm, km, vm = q[:, :N_META], k[:, :N_META], v[:, :N_META]
    mi = np.arange(N_META)
    dist_m = mi[:, None] - mi[None, :]
    bias_m = jnp.transpose(bias_tab[t5_bucket(dist_m)], (2, 3, 0, 1))
    lm = jnp.einsum('bqhgd,bkhd->bhgqk', qm, km).astype(jnp.float32) + bias_m
    lm = jnp.where(dist_m >= 0, lm, NEG)
    om = jnp.einsum('bhgqk,bkhd->bqhgd', sink_softmax(lm, sink).astype(v.dtype), vm)
    qr = q[:, N_META:].reshape(b, nb, ATTN_BLOCK, ATTN_KV_HEADS, ATTN_GROUP, HEAD_DIM)
    kr = k[:, N_META:].reshape(b, nb, ATTN_BLOCK, ATTN_KV_HEADS, HEAD_DIM)
    vr = v[:, N_META:].reshape(b, nb, ATTN_BLOCK, ATTN_KV_HEADS, HEAD_DIM)

    def band_keys(t, tm):
        prev = jnp.concatenate([jnp.zeros_like(t[:, :1]), t[:, :-1]], axis=1)
        meta = jnp.broadcast_to(tm[:, None], (b, nb, N_META, ATTN_KV_HEADS, HEAD_DIM))
        return jnp.concatenate([meta, prev, t], axis=2)

    kb, vb = band_keys(kr, km), band_keys(vr, vm)
    bucket, valid = band_structure(nb)
    bias_b = jnp.transpose(bias_tab[bucket], (0, 3, 4, 1, 2))
    lr = jnp.einsum('bnqhgd,bnkhd->bnhgqk', qr, kb).astype(jnp.float32) + bias_b
    lr = jnp.where(valid[:, None, None], lr, NEG)
    orr = jnp.einsum('bnhgqk,bnkhd->bnqhgd', sink_softmax(lr, sink).astype(v.dtype), vb)
    return jnp.concatenate([om.reshape(b, N_META, D_ATTN), orr.reshape(b, S, D_ATTN)], axis=1)


def ssd_mixer(z, xbc, dt_raw, conv_w, conv_b, dt_bias, a_log, d_skip, norm_w):
    b, L, _ = xbc.shape
    xbc = lax.conv_general_dilated(xbc, conv_w[:, None, :], window_strides=(1,),
                                   padding=[(CONV_WIDTH - 1, 0)],
                                   dimension_numbers=('NWC', 'WIO', 'NWC'),
                                   feature_group_count=D_XBC) + conv_b
    xbc = jax.nn.silu(xbc)
    xs, Bm, Cm = jnp.split(xbc, [SSM_D_INNER, SSM_D_INNER + SSM_GROUPS * SSM_STATE], axis=-1)
    dt = jax.nn.softplus(dt_raw.astype(jnp.float32) + dt_bias.astype(jnp.float32))
    A = -jnp.exp(a_log.astype(jnp.float32)).reshape(SSM_GROUPS, SSM_HPG)
    pad = (-N_META) % SSD_CHUNK
    padf = lambda t: jnp.pad(t, ((0, 0), (pad, 0)) + ((0, 0),) * (t.ndim - 2))
    Lp = L + pad
    nc = Lp // SSD_CHUNK
    x = padf(xs).reshape(b, nc, SSD_CHUNK, SSM_GROUPS, SSM_HPG, SSM_HEAD_DIM)
    Bc = padf(Bm).reshape(b, nc, SSD_CHUNK, SSM_GROUPS, SSM_STATE)
    Cc = padf(Cm).reshape(b, nc, SSD_CHUNK, SSM_GROUPS, SSM_STATE)
    dtc = padf(dt).reshape(b, nc, SSD_CHUNK, SSM_GROUPS, SSM_HPG)
    xdt = x * dtc[..., None].astype(x.dtype)
    dA = jnp.transpose(dtc * A, (0, 3, 4, 1, 2))
    A_cs = jnp.cumsum(dA, axis=-1)
    tri = np.tril(np.ones((SSD_CHUNK, SSD_CHUNK), bool))
    Lmat = jnp.exp(jnp.where(tri, A_cs[..., :, None] - A_cs[..., None, :], -jnp.inf))
    CB = jnp.einsum('bclgn,bcsgn->bgcls', Cc, Bc)
    M = CB[:, :, None] * Lmat
    y_diag = jnp.einsum('bghcls,bcsghp->bclghp', M, xdt)
    decay_states = jnp.exp(A_cs[..., -1:] - A_cs)
    states = jnp.einsum('bcsgn,bghcs,bcsghp->bcghpn', Bc, decay_states, xdt).astype(jnp.float32)
    chunk_decay = jnp.exp(A_cs[..., -1])

    def step(h, inp):
        st, dec = inp
        return dec[..., None, None] * h + st, h

    h0 = jnp.zeros((b, SSM_GROUPS, SSM_HPG, SSM_HEAD_DIM, SSM_STATE), jnp.float32)
    _, states_prev = lax.scan(step, h0, (jnp.moveaxis(states, 1, 0), jnp.moveaxis(chunk_decay, -1, 0)))
    y_off = jnp.einsum('bclgn,bghcl,cbghpn->bclghp', Cc, jnp.exp(A_cs), states_prev)
    y = y_diag + y_off + x * d_skip.reshape(SSM_GROUPS, SSM_HPG)[..., None]
    y = y.reshape(b, Lp, SSM_D_INNER)[:, pad:].astype(xs.dtype)
    yg = (y * jax.nn.silu(z)).astype(jnp.float32).reshape(b, L, SSM_GROUPS, -1)
    yg = yg * lax.rsqrt(jnp.mean(yg * yg, axis=-1, keepdims=True) + EPS)
    return (yg.reshape(b, L, SSM_D_INNER) * norm_w.astype(jnp.float32)).astype(z.dtype)


def mixer_layer(hn, w_in, sinks, rel_bias, conv_w, conv_b, dt_bias, a_log, d_skip,
                attn_norm_w, ssm_norm_w, w_out):
    proj = hn @ w_in
    q, k, v, z, xbc, dt_raw = jnp.split(proj, list(IN_SPLITS), axis=-1)
    ya = rmsnorm(swa_sink_attention(q, k, v, sinks, rel_bias), attn_norm_w)
    ys = ssd_mixer(z, xbc, dt_raw, conv_w, conv_b, dt_bias, a_log, d_skip, ssm_norm_w)
    return jnp.concatenate([ya, ys], axis=-1) @ w_out


def peer_ffn(xn, wq, keys, u, v):
    b, t, d = xn.shape
    T = b * t
    xt = xn.reshape(T, d)
    q = (xt @ wq).reshape(T, PEER_HEADS, 2, PEER_HALF)
    s = jnp.einsum('thcd,hckd->thck', q, keys).astype(jnp.float32)
    s1, i1 = lax.top_k(s[:, :, 0], PEER_TOPK)
    s2, i2 = lax.top_k(s[:, :, 1], PEER_TOPK)
    cand = (s1[..., :, None] + s2[..., None, :]).reshape(T, PEER_HEADS, PEER_TOPK * PEER_TOPK)
    top, pos = lax.top_k(cand, PEER_TOPK)
    idx = (jnp.take_along_axis(i1, pos // PEER_TOPK, axis=-1) * N_KEYS
           + jnp.take_along_axis(i2, pos % PEER_TOPK, axis=-1))
    gates = jax.nn.softmax(top, axis=-1)
    nblk = -(-T // PEER_BLOCK)
    padT = nblk * PEER_BLOCK - T
    xb = jnp.pad(xt, ((0, padT), (0, 0))).reshape(nblk, PEER_BLOCK, d)
    ib = jnp.pad(idx, ((0, padT), (0, 0), (0, 0))).reshape(nblk, PEER_BLOCK, PEER_HEADS, PEER_TOPK)
    gb = jnp.pad(gates, ((0, padT), (0, 0), (0, 0))).reshape(nblk, PEER_BLOCK, PEER_HEADS, PEER_TOPK)

    def block(args):
        x_blk, i_blk, g_blk = args
        a = jnp.einsum('thkd,td->thk', u[i_blk], x_blk).astype(jnp.float32)
        w = (g_blk * jax.nn.gelu(a, approximate=False)).astype(x_blk.dtype)
        return jnp.einsum('thk,thkd->td', w, v[i_blk])

    out = lax.map(block, (xb, ib, gb)).reshape(nblk * PEER_BLOCK, d)[:T]
    return out.reshape(b, t, d)


def setup_inputs(seed: int = 0) -> dict:
    key = jax.random.key(seed)
    ks = jax.random.split(key, 20)
    f32 = jnp.float32
    nrm = lambda k, shape, scale: jax.random.normal(k, shape, f32) * scale
    dt0 = jnp.exp(jax.random.uniform(ks[8], (DEPTH, SSM_HEADS), f32, np.log(1e-3), np.log(1e-1)))
    return {
        "x": nrm(ks[0], (BATCH, SEQ, D_MODEL), 1.0),
        "meta_tokens": nrm(ks[1], (N_META, D_MODEL), 1.0),
        "rel_bias": nrm(ks[2], (REL_BUCKETS, ATTN_HEADS), 0.5),
        "ln_mix": 1.0 + nrm(ks[3], (DEPTH, D_MODEL), 0.02),
        "w_in": nrm(ks[4], (DEPTH, D_MODEL, D_IN_PROJ), D_MODEL ** -0.5),
        "attn_sinks": nrm(ks[5], (DEPTH, ATTN_HEADS), 0.5),
        "conv_w": nrm(ks[6], (DEPTH, CONV_WIDTH, D_XBC), CONV_WIDTH ** -0.5),
        "conv_b": nrm(ks[7], (DEPTH, D_XBC), 0.02),
        "dt_bias": dt0 + jnp.log(-jnp.expm1(-dt0)),
        "a_log": jnp.log(jax.random.uniform(ks[9], (DEPTH, SSM_HEADS), f32, 1.0, 16.0)),
        "d_skip": 1.0 + nrm(ks[10], (DEPTH, SSM_HEADS), 0.02),
        "attn_norm_w": 1.0 + nrm(ks[11], (DEPTH, D_ATTN), 0.02),
        "ssm_norm_w": 1.0 + nrm(ks[12], (DEPTH, SSM_D_INNER), 0.02),
        "w_out": nrm(ks[13], (DEPTH, D_MIX, D_MODEL), D_MIX ** -0.5),
        "ln_ffn": 1.0 + nrm(ks[14], (DEPTH, D_MODEL), 0.02),
        "peer_wq": nrm(ks[15], (DEPTH, D_MODEL, PEER_HEADS * PEER_KEY_DIM), D_MODEL ** -0.5),
        "peer_keys": nrm(ks[16], (DEPTH, PEER_HEADS, 2, N_KEYS, PEER_HALF), PEER_HALF ** -0.5),
        "peer_u": nrm(ks[17], (DEPTH, N_EXPERTS, D_MODEL), D_MODEL ** -0.5),
        "peer_v": nrm(ks[18], (DEPTH, N_EXPERTS, D_MODEL), PEER_HEADS ** -0.5),
        "ln_final": 1.0 + nrm(ks[19], (D_MODEL,), 0.02),
    }


def reference(x, meta_tokens, rel_bias, ln_mix, w_in, attn_sinks, conv_w, conv_b, dt_bias, a_log,
              d_skip, attn_norm_w, ssm_norm_w, w_out, ln_ffn, peer_wq, peer_keys, peer_u, peer_v,
              ln_final):
    b = x.shape[0]
    meta = jnp.broadcast_to(meta_tokens[None].astype(x.dtype), (b, N_META, D_MODEL))
    h = jnp.concatenate([meta, x], axis=1)
    for layer in range(DEPTH):
        h = h + mixer_layer(rmsnorm(h, ln_mix[layer]), w_in[layer], attn_sinks[layer], rel_bias,
                            conv_w[layer], conv_b[layer], dt_bias[layer], a_log[layer],
                            d_skip[layer], attn_norm_w[layer], ssm_norm_w[layer], w_out[layer])
        if layer == DEPTH - 1:
            h = h[:, N_META:]
        h = h + peer_ffn(rmsnorm(h, ln_ffn[layer]), peer_wq[layer], peer_keys[layer],
                         peer_u[layer], peer_v[layer])
    return rmsnorm(h, ln_final)
```

```python
import numpy as np
from contextlib import ExitStack
import concourse.bass as bass
import concourse.mybir as mybir
from concourse.bass_utils import run_bass_kernel_spmd

F32 = mybir.dt.float32
BF16 = mybir.dt.bfloat16
AF = mybir.ActivationFunctionType
ALU = mybir.AluOpType
AX = mybir.AxisListType

SAME_ENGINE_SYNC = True
NDMA_SEM = 8
NCORES = 8
D = 2048
NPRE = 9
NMAIN = 8
EPS = 1e-6
NEG = -1e30
DELTA = 1e-5
IA = 4
NW = 3


class Prog:
    CMP = ("pe", "act", "dve", "pool")

    def __init__(self, nc):
        self.nc = nc
        self.streams = {e: [] for e in ("pe", "act", "dve", "pool", "sync")}
        self.cnt = {e: 0 for e in self.CMP}
        self.sem = {}
        self.dma_sems = {}
        self.dma_n = {}
        self.waited = {e: {} for e in self.streams}
        self.last_w = {}
        self.readers = {}

    def setup(self, stack):
        nc = self.nc
        for e in self.CMP:
            self.sem[e] = stack.enter_context(nc.semaphore("c_" + e))
        for q in ("sync", "act", "pool"):
            self.dma_sems[q] = [stack.enter_context(nc.semaphore("d_%s%d" % (q, i))) for i in range(NDMA_SEM)]
            self.dma_n[q] = 0

    def _deps(self, reads, writes):
        toks = []
        for k in reads:
            t = self.last_w.get(k)
            if t is not None:
                toks.append(t + (0,))
        for k in writes:
            t = self.last_w.get(k)
            if t is not None:
                toks.append(t + (1,))
            for t in self.readers.get(k, ()):
                toks.append(t + (1,))
        return toks

    def _commit(self, tok, reads, writes):
        for k in reads:
            self.readers.setdefault(k, []).append(tok)
        for k in writes:
            self.last_w[k] = tok
            self.readers[k] = []

    def _waits(self, stream, toks, is_dma=False):
        need = {}
        for (sk, val, src, kind) in toks:
            if (not is_dma) and src == stream:
                if stream == "pe" or not SAME_ENGINE_SYNC or kind == 1:
                    continue
            if need.get(sk, 0) < val:
                need[sk] = val
        out = []
        w = self.waited[stream]
        for sk, val in need.items():
            if w.get(sk, 0) >= val:
                continue
            w[sk] = val
            out.append((sk, val))
        return out

    def _semh(self, sk):
        if isinstance(sk, str):
            return self.sem[sk]
        return self.dma_sems[sk[0]][sk[1]]

    def op(self, eng, fn, reads=(), writes=(), inc=True):
        toks = self._deps(reads, writes)
        waits = self._waits(eng, toks)
        if inc:
            self.cnt[eng] += 1
            tok = (eng, self.cnt[eng], eng)
            self.streams[eng].append((waits, fn, (eng, 1)))
        else:
            tok = (eng, self.cnt[eng] + 1, eng)
            self.streams[eng].append((waits, fn, None))
        self._commit(tok, reads, writes)

    def dma(self, q, out, in_, reads=(), writes=()):
        toks = self._deps(reads, writes)
        n = self.dma_n[q]
        self.dma_n[q] += 1
        si = n % NDMA_SEM
        rnd = n // NDMA_SEM
        sk = (q, si)
        if rnd > 0:
            toks.append((sk, 16 * rnd, "dma", 1))
        waits = self._waits(q, toks, is_dma=True)
        tok = (sk, 16 * (rnd + 1), "dma")

        def fn(e, out=out, in_=in_):
            return e.dma_start(out=out, in_=in_)

        self.streams[q].append((waits, fn, (sk, 16)))
        self._commit(tok, reads, writes)

    def _all_tokens(self):
        need = []
        for q in self.dma_sems:
            n = self.dma_n[q]
            for si in range(NDMA_SEM):
                cnt = (n - si + NDMA_SEM - 1) // NDMA_SEM if n > si else 0
                if cnt > 0:
                    need.append(((q, si), 16 * cnt))
        for e in self.CMP:
            if self.cnt[e] > 0:
                need.append((e, self.cnt[e]))
        return need

    def barrier(self):
        need = self._all_tokens()
        for s in self.streams:
            w = self.waited[s]
            ws = []
            for sk, val in need:
                if w.get(sk, 0) >= val:
                    continue
                w[sk] = val
                ws.append((sk, val))
            if ws:
                self.streams[s].append((ws, None, None))
        self.last_w = {}
        self.readers = {}

    def emit(self):
        nc = self.nc
        prog = self
        streams = self.streams
        self.streams = {e: [] for e in streams}
        with nc.Block() as block:
            def run(stream, e):
                for (waits, fn, inc) in streams[stream]:
                    for (sk, val) in waits:
                        e.wait_ge(prog._semh(sk), val)
                    if fn is None:
                        continue
                    ins = fn(e)
                    if inc is not None:
                        ins.then_inc(prog._semh(inc[0]), inc[1])

            @block.tensor
            def _(e):
                run("pe", e)

            @block.scalar
            def _(e):
                run("act", e)

            @block.vector
            def _(e):
                run("dve", e)

            @block.gpsimd
            def _(e):
                run("pool", e)

            @block.sync
            def _(e):
                run("sync", e)


def build_nc():
    nc = bass.Bass("TRN2", target_bir_lowering=False)
    din = lambda name, shape: nc.dram_tensor(name, list(shape), F32, kind="ExternalInput").ap()
    xtok = din("xtok", [(NPRE + NMAIN) * 128, D])
    xmeta = din("xmeta", [128, D])
    padmask = din("padmask", [128, NPRE])
    w_in_blk = din("w_in_blk", [8, 128, 16, 512])
    w_dt = din("w_dt", [128, 16, 16])
    w_out_blk = din("w_out_blk", [4, 128, 16, 512])
    wq_blk = din("wq_blk", [16, 128, 16, 128])
    keysT = din("keysT", [128, 16, 128])
    u_blk = din("u_blk", [128, 128, 16, 128])
    v_blk = din("v_blk", [128, 128, D])
    convw_r = din("convw_r", [128, 4, 1536])
    convb_r = din("convb_r", [1, 1536])
    hv = din("hv", [128, 4, 16])
    nw = din("nw", [128, 3, 16])
    lnfin = din("lnfin", [128, D])
    cm = din("cm", [128, 4, 128])
    shm_r = din("shm_r", [128, 7, 128])
    bias_gen = din("bias_gen", [3, 4, 128, 512])
    bias_c0 = din("bias_c0", [2, 4, 128, 512])
    out = nc.dram_tensor("out", [NMAIN * 128, D], F32, kind="ExternalOutput").ap()
    h2s = nc.dram_tensor("h2s", [NMAIN * 128, D], F32, kind="Internal").ap()
    wsc = nc.dram_tensor("wsc", [12, 128, 16, 512], BF16, kind="Internal").ap()

    with ExitStack() as st0:
        P = Prog(nc)
        P.setup(st0)

        def OP(eng, fn, r=(), w=()):
            P.op(eng, fn, reads=r, writes=w)

        def MM(out, lhsT, rhs, start, stop, r, w):
            P.op("pe", lambda e: e.matmul(out=out, lhsT=lhsT, rhs=rhs, start=start, stop=stop), reads=r, writes=w,
                 inc=bool(stop))

        def TR(out, in_, ident, r, w):
            P.op("pe", lambda e: e.transpose(out=out, in_=in_, identity=ident), reads=r, writes=w)

        def ACT(out, in_, func, r, w, bias=None, scale=None, accum=None, alpha=None):
            kw = {}
            if alpha is not None:
                kw["alpha"] = alpha
            if bias is not None:
                kw["bias"] = bias
            if scale is not None:
                kw["scale"] = scale
            if accum is not None:
                kw["accum_out"] = accum
            P.op("act", lambda e: e.activation(out=out, in_=in_, func=func, **kw), reads=r, writes=w)

        def TT(out, in0, in1, op, r, w, eng="dve"):
            P.op(eng, lambda e: e.tensor_tensor(out=out, in0=in0, in1=in1, op=op), reads=r, writes=w)

        def TS(out, in0, s1, s2, op0, op1, r, w, eng="dve"):
            if s2 is None:
                P.op(eng, lambda e: e.tensor_scalar(out=out, in0=in0, scalar1=s1, scalar2=None, op0=op0),
                     reads=r, writes=w)
            else:
                P.op(eng, lambda e: e.tensor_scalar(out=out, in0=in0, scalar1=s1, scalar2=s2, op0=op0, op1=op1),
                     reads=r, writes=w)

        def STT(out, in0, scalar, in1, op0, op1, r, w, eng="dve"):
            P.op(eng, lambda e: e.scalar_tensor_tensor(out=out, in0=in0, scalar=scalar, in1=in1, op0=op0, op1=op1),
                 reads=r, writes=w)

        def CP(out, in_, r, w, eng="dve"):
            if eng == "act":
                P.op("act", lambda e: e.copy(out=out, in_=in_), reads=r, writes=w)
            else:
                P.op(eng, lambda e: e.tensor_copy(out=out, in_=in_), reads=r, writes=w)

        def RECIP(out, in_, r, w):
            P.op("dve", lambda e: e.reciprocal(out=out, in_=in_), reads=r, writes=w)

        sbp = lambda name, shape, dt: st0.enter_context(nc.sbuf_tensor(name, shape, dt))
        cmt = sbp("cmt", [128, 4, 128], F32)
        idb = sbp("idb", [128, 128], BF16)
        nwt = sbp("nwt", [128, 3, 16], F32)
        ident_f = cmt[:, 0, :]
        triU = cmt[:, 1, :]
        SLm = cmt[:, 2, :]
        ones_f = cmt[:, 3, :]
        P.dma("sync", cmt[:], cm, writes=["cmt"])
        P.dma("sync", nwt[:], nw, writes=["nwt"])
        CP(idb[:], cmt[:, 0, :], ["cmt"], ["idb"])

        def rms_rstd(xin, ss, junk, n, r, key, jkey=None):
            ACT(junk, xin, AF.Square, r, [jkey or (key + "_junk"), key + "_ss"], accum=ss[:, 0:1])
            ACT(ss[:, 1:2], ss[:, 0:1], AF.Ln, [key + "_ss"], [key + "_ss"], bias=EPS, scale=1.0 / n)
            ACT(ss[:, 2:3], ss[:, 1:2], AF.Exp, [key + "_ss"], [key + "_ss"], scale=-0.5)

        with ExitStack() as st1:
            sb = lambda name, shape, dt: st1.enter_context(nc.sbuf_tensor(name, shape, dt))
            pA = st1.enter_context(nc.psum_tensor("pA", [128, 512], F32))
            pB = st1.enter_context(nc.psum_tensor("pB", [128, 512], F32))
            pS = st1.enter_context(nc.psum_tensor("pS", [128, 512], F32))
            pT = st1.enter_context(nc.psum_tensor("pT", [128, 1024], BF16))
            pBig = st1.enter_context(nc.psum_tensor("pBig", [128, 2048], F32))
            pAB = [pA, pB]

            x_t = [sb("x_t%d" % i, [128, D], F32) for i in range(2)]
            junk = sb("junk", [128, D], BF16)
            ss = sb("ss", [128, 8], F32)
            hn = sb("hn", [128, D], BF16)
            tT = sb("tT", [128, 16, 128], BF16)
            wbuf = [sb("wbuf%d" % i, [128, 16, 512], BF16) for i in range(NW)]
            wdt = sb("wdt", [128, 16, 16], BF16)
            q_sb = sb("q_sb", [128, 1024], BF16)
            kv_sb = sb("kv_sb", [128, 512], BF16)
            xw = [[sb("xw%d_%d" % (pp, j), [128, 1536], BF16) for j in range(4)] for pp in range(2)]
            convw = sb("convw", [128, 4, 1536], BF16)
            convb = sb("convb", [1, 1536], BF16)
            ones_row = sb("ones_row", [1, 128], BF16)
            shm = sb("shm", [128, 7, 128], BF16)
            hvt = sb("hvt", [128, 4, 16], F32)
            pmask = sb("pmask", [128, NPRE], F32)
            xs_f = sb("xs_f", [128, 1024], F32)
            bc_b = sb("bc_b", [128, 512], BF16)
            zs = sb("zs", [128, 1024], BF16)
            sm = sb("sm", [128, 8, 16], F32)
            dec = sb("dec", [128, 48], F32)
            dAU = [sb("dAU%d" % i, [128, 4, 128], F32) for i in range(2)]
            LT = sb("LT", [128, 16, 128], BF16)
            bcT = sb("bcT", [128, 4, 128], BF16)
            cbm = sb("cbm", [128, 2, 128], BF16)
            xdt = sb("xdt", [128, 1024], BF16)
            xdtw = sb("xdtw", [128, 1024], BF16)
            scr = [sb("scr%d" % i, [128, 1024], F32) for i in range(2)]
            S_f = sb("S_f", [128, 1024], F32)
            S_b = sb("S_b", [128, 1024], BF16)
            QT = sb("QT", [128, 16, 128], BF16)
            KT = [sb("KT%d" % i, [128, 4, 128], BF16) for i in range(3)]
            vext = [sb("vext%d" % i, [128, 4, 65], BF16) for i in range(3)]
            biasb = [sb("biasb%d" % i, [128, 512], F32) for i in range(3)]
            lgt = [sb("lgt%d" % i, [128, 512], F32) for i in range(3)]
            PT = [sb("PT%d" % i, [128, 4, 128], BF16) for i in range(3)]
            ao = sb("ao", [128, 16, 65], F32)
            esink = sb("esink", [128, 16], F32)
            Aneg = sb("Aneg", [128, 16], F32)

            P.dma("pool", convw[:], convw_r, writes=["convw"])
            P.dma("pool", convb[:], convb_r, writes=["convb"])
            P.dma("pool", shm[:], shm_r, writes=["shm"])
            P.dma("sync", hvt[:], hv, writes=["hvt"])
            P.dma("sync", pmask[:], padmask, writes=["pmask"])
            OP("dve", lambda e: e.memset(ones_row[:], 1.0), [], ["ones_row"])
            for i in range(3):
                OP("dve", lambda e, i=i: e.memset(vext[i][:, :, 64:65], 1.0), [], ["vext%d" % i])
            OP("dve", lambda e: e.memset(S_f[:], 0.0), [], ["S_f"])
            OP("dve", lambda e: e.memset(S_b[:], 0.0), [], ["S_b"])
            for j in range(4):
                OP("dve", lambda e, j=j: e.memset(xw[1][j][:], 0.0), [], ["xw1_%d" % j])
            ACT(Aneg[:], hvt[:, 1, :], AF.Exp, ["hvt"], ["Aneg"])
            TS(Aneg[:], Aneg[:], -1.0, None, ALU.mult, None, ["Aneg"], ["Aneg"])
            ACT(esink[:], hvt[:, 3, :], AF.Exp, ["hvt"], ["esink"])
            dtb = hvt[:, 0, :]
            dskip = hvt[:, 2, :]

            wseq = [2] + [5, 6, 7] * (NPRE - 1) + [5, 6, 7, 2] + [5, 6, 7, 3, 4, 0, 1, 2, 8, 9, 10, 11] * NMAIN
            wstate = {"use": 0, "iss": 0, "seen": set()}

            def issue_w():
                k = wstate["iss"]
                if k >= len(wseq):
                    return
                wstate["iss"] += 1
                tid = wseq[k]
                i = k % NW
                if tid not in wstate["seen"]:
                    wstate["seen"].add(tid)
                    src = w_in_blk[tid] if tid < 8 else w_out_blk[tid - 8]
                    P.dma("pool", wbuf[i][:], src, writes=["wbuf%d" % i])
                    P.dma("sync", wsc[tid], wbuf[i][:], reads=["wbuf%d" % i], writes=["wsc%d" % tid])
                else:
                    P.dma("sync", wbuf[i][:], wsc[tid], reads=["wsc%d" % tid], writes=["wbuf%d" % i])

            def load_w(tid):
                k = wstate["use"]
                assert wseq[k] == tid, (k, wseq[k], tid)
                wstate["use"] += 1
                return k % NW

            xseq = [xmeta] + [xtok[ci * 128:(ci + 1) * 128, :] for ci in range(NPRE + NMAIN)]
            xstate = {"n": 0}

            def issue_x(k):
                if k < len(xseq):
                    P.dma("sync", x_t[k % 2][:], xseq[k], writes=["x_t%d" % (k % 2)])

            def norm_T(nwi):
                k = xstate["n"]
                xstate["n"] += 1
                xb = k % 2
                rms_rstd(x_t[xb][:], ss, junk[:], D, ["x_t%d" % xb], "m")
                TS(hn[:], x_t[xb][:], ss[:, 2:3], None, ALU.mult, None, ["x_t%d" % xb, "m_ss"], ["hn"])
                transpose16(hn, nwi, "hn")
                return xb

            def transpose16(src, nwi, key):
                for half in range(2):
                    for kk in range(8):
                        k = half * 8 + kk
                        TR(pT[:, kk * 128:(kk + 1) * 128], src[:, k * 128:(k + 1) * 128], idb[:], [key, "idb"], ["pT"])
                    for kk in range(8):
                        k = half * 8 + kk
                        TS(tT[:, k, :], pT[:, kk * 128:(kk + 1) * 128], nwt[:, nwi, k:k + 1], None, ALU.mult, None,
                           ["pT", "nwt"], ["tT"], eng=("dve" if kk % 2 == 0 else "dve"))

            mmc = {"n": 0}

            def proj_tile(wi, ncols=512):
                b = mmc["n"] % 2
                mmc["n"] += 1
                pk = "pAB%d" % b
                for k in range(16):
                    MM(pAB[b][:, 0:ncols], tT[:, k, :], wbuf[wi][:, k, 0:ncols], k == 0, k == 15,
                       ["tT", "wbuf%d" % wi], [pk])
                issue_w()
                return pAB[b], pk

            def kv_to_attn(slot):
                for kv in range(4):
                    TR(pT[0:64, kv * 128:(kv + 1) * 128], kv_sb[:, kv * 64:(kv + 1) * 64], idb[:], ["kv_sb", "idb"], ["pT"])
                CP(KT[slot][0:64, :, :], pT[0:64, 0:512].rearrange("p (a t) -> p a t", a=4), ["pT"], ["KT%d" % slot])
                CP(vext[slot][:, :, 0:64], kv_sb[:, 256:512].rearrange("p (a d) -> p a d", a=4), ["kv_sb"],
                   ["vext%d" % slot], eng="act")

            cstate = {}

            def chunk(ci, mode, par):
                main = mode == "main"
                xb = norm_T(0)
                cstate["xb"] = xb
                for tix, c0 in ((5, 0), (6, 512), (7, 1024)):
                    wi = load_w(tix)
                    pt, pk = proj_tile(wi)
                    for j in range(4):
                        TT(xw[par][j][:, c0:c0 + 512], pt[:], convw[:, j, c0:c0 + 512], ALU.mult,
                           [pk, "convw"], ["xw%d_%d" % (par, j)])
                P.dma("pool", wdt[:], w_dt, writes=["wdt"]) if ci == 0 else None
                for k in range(16):
                    MM(pS[:, 0:16], tT[:, k, :], wdt[:, k, :], k == 0, k == 15, ["tT", "wdt"], ["pS"])
                TT(sm[:, 0, :], pS[:, 0:16], dtb, ALU.add, ["pS", "hvt"], ["sm0"])
                ACT(sm[:, 0, :], sm[:, 0, :], AF.Exp, ["sm0"], ["sm0"])
                ACT(sm[:, 0, :], sm[:, 0, :], AF.Ln, ["sm0"], ["sm0"], bias=1.0)
                if not main:
                    TS(sm[:, 0, :], sm[:, 0, :], pmask[:, ci:ci + 1], None, ALU.mult, None, ["sm0", "pmask"], ["sm0"])
                dt = sm[:, 0, :]
                TT(sm[:, 1, :], dt, Aneg[:], ALU.mult, ["sm0", "Aneg"], ["sm1"])
                dA = sm[:, 1, :]
                def rest_tiles():
                    if main:
                        for tix, c0 in ((3, 0), (4, 512)):
                            wi = load_w(tix)
                            pt, pk = proj_tile(wi)
                            ACT(zs[:, c0:c0 + 512], pt[:], AF.Silu, [pk], ["zs"])
                            yield
                        for tix, c0 in ((0, 0), (1, 512)):
                            wi = load_w(tix)
                            pt, pk = proj_tile(wi)
                            CP(q_sb[:, c0:c0 + 512], pt[:], [pk], ["q_sb"], eng="act")
                            yield
                    if main or mode == "pre_kv":
                        wi = load_w(2)
                        pt, pk = proj_tile(wi)
                        CP(kv_sb[:], pt[:], [pk], ["kv_sb"], eng="act")
                        yield

                rest = rest_tiles()

                def adv():
                    next(rest, None)

                def drain():
                    for _ in rest:
                        pass

                issue_x(xstate["n"])
                for cb in range(3):
                    c0 = cb * 512
                    pk = "pBig%d" % cb
                    o = pBig[:, c0:c0 + 512]
                    for j in range(4):
                        MM(o, shm[:, j, :], xw[par][j][:, c0:c0 + 512], j == 0, False, ["shm", "xw%d_%d" % (par, j)], [pk])
                    for j in range(3):
                        MM(o, shm[:, 4 + j, :], xw[1 - par][j][:, c0:c0 + 512], False, False,
                           ["shm", "xw%d_%d" % (1 - par, j)], [pk])
                    MM(o, ones_row[0:1, :], convb[0:1, c0:c0 + 512], False, True, ["ones_row", "convb"], [pk])
                adv()
                ACT(xs_f[:, 0:512], pBig[:, 0:512], AF.Silu, ["pBig0"], ["xs_f"])
                ACT(xs_f[:, 512:1024], pBig[:, 512:1024], AF.Silu, ["pBig1"], ["xs_f"])
                ACT(bc_b[:], pBig[:, 1024:1536], AF.Silu, ["pBig2"], ["bc_b"])
                MM(pS[:, 16:32], SLm, dA, True, True, ["cmt", "sm1"], ["pS"])
                MM(pS[:, 32:48], ones_f, dA, True, True, ["cmt", "sm1"], ["pS"])
                MM(pS[:, 48:64], triU, dA, True, True, ["cmt", "sm1"], ["pS"])
                adv()
                ACT(dec[:], pS[:, 16:64], AF.Exp, ["pS"], ["dec"])
                Lend, cdec, ecs = dec[:, 0:16], dec[:, 16:32], dec[:, 32:48]
                xs3 = xs_f[:].rearrange("p (h d) -> p h d", h=16)
                if main:
                    for qd in range(4):
                        bi = qd % 2
                        TT(dAU[bi][:], dA[:, qd * 4:(qd + 1) * 4].unsqueeze(2).to_broadcast([128, 4, 128]),
                           triU.unsqueeze(1).to_broadcast([128, 4, 128]), ALU.mult, ["sm1", "cmt"], ["dAU%d" % bi])
                        MM(pBig[:, qd * 512:(qd + 1) * 512], SLm, dAU[bi][:].rearrange("p a l -> p (a l)"), True, True,
                           ["cmt", "dAU%d" % bi], ["pBig%d" % qd])
                        adv()
                        ACT(LT[:, qd * 4:(qd + 1) * 4, :].rearrange("p a l -> p (a l)"), pBig[:, qd * 512:(qd + 1) * 512],
                            AF.Exp, ["pBig%d" % qd], ["LT"])
                    for a in range(4):
                        TR(pT[:, a * 128:(a + 1) * 128], bc_b[:, a * 128:(a + 1) * 128], idb[:], ["bc_b", "idb"], ["pT"])
                    CP(bcT[:].rearrange("p a t -> p (a t)"), pT[:, 0:512], ["pT"], ["bcT"])
                    bb = mmc["n"] % 2
                    mmc["n"] += 1
                    for g in range(2):
                        MM(pAB[bb][:, g * 128:(g + 1) * 128], bcT[:, g, :], bcT[:, 2 + g, :], True, True, ["bcT"], ["pAB%d" % bb])
                    TT(cbm[:], pAB[bb][:, 0:256].rearrange("p (g l) -> p g l", g=2),
                       triU.unsqueeze(1).to_broadcast([128, 2, 128]), ALU.mult, ["pAB%d" % bb, "cmt"], ["cbm"])
                    for g in range(2):
                        TT(LT[:, 8 * g:8 * g + 8, :], LT[:, 8 * g:8 * g + 8, :],
                           cbm[:, g, :].unsqueeze(1).to_broadcast([128, 8, 128]), ALU.mult, ["LT", "cbm"], ["LT"])
                    TT(xdt[:].rearrange("p (h d) -> p h d", h=16), xs3, dt.unsqueeze(2).to_broadcast([128, 16, 64]),
                       ALU.mult, ["xs_f", "sm0"], ["xdt"])
                    for h in range(16):
                        MM(pBig[:, h * 64:(h + 1) * 64], LT[:, h, :], xdt[:, h * 64:(h + 1) * 64], True, True,
                           ["LT", "xdt"], ["pBig%d" % (h // 8)])
                    for g in range(2):
                        MM(pBig[:, 1024 + g * 512:1024 + (g + 1) * 512], bcT[:, 2 + g, :], S_b[:, g * 512:(g + 1) * 512],
                           True, True, ["bcT", "S_b"], ["pBig%d" % (2 + g)])
                    CP(scr[0][:], pBig[:, 1024:2048], ["pBig2", "pBig3"], ["scr0"], eng="act")
                    s03 = scr[0][:].rearrange("p (h d) -> p h d", h=16)
                    s13 = scr[1][:].rearrange("p (h d) -> p h d", h=16)
                    TT(s03, s03, ecs.unsqueeze(2).to_broadcast([128, 16, 64]), ALU.mult, ["scr0", "dec"], ["scr0"])
                    TT(scr[0][:], scr[0][:], pBig[:, 0:1024], ALU.add, ["scr0", "pBig0", "pBig1"], ["scr0"])
                    TT(s13, xs3, dskip.unsqueeze(2).to_broadcast([128, 16, 64]), ALU.mult, ["xs_f", "hvt"], ["scr1"])
                    TT(scr[0][:], scr[0][:], scr[1][:], ALU.add, ["scr0", "scr1"], ["scr0"])
                    drain()
                    TT(scr[0][:], scr[0][:], zs[:], ALU.mult, ["scr0", "zs"], ["scr0"])
                    for g in range(2):
                        ACT(junk[:, 0:512], scr[0][:, g * 512:(g + 1) * 512], AF.Square, ["scr0"], ["m_junk", "g_ss"],
                            accum=ss[:, 3 + g:4 + g])
                    ACT(ss[:, 5:7], ss[:, 3:5], AF.Ln, ["g_ss"], ["g_ss"], bias=EPS, scale=1.0 / 512)
                    ACT(ss[:, 5:7], ss[:, 5:7], AF.Exp, ["g_ss"], ["g_ss"], scale=-0.5)
                    for g in range(2):
                        TS(hn[:, 1024 + g * 512:1024 + (g + 1) * 512], scr[0][:, g * 512:(g + 1) * 512], ss[:, 5 + g:6 + g],
                           None, ALU.mult, None, ["scr0", "g_ss"], ["hn"])
                TT(sm[:, 2, :], dt, Lend, ALU.mult, ["sm0", "dec"], ["sm2"])
                TT(xdtw[:].rearrange("p (h d) -> p h d", h=16), xs3, sm[:, 2, :].unsqueeze(2).to_broadcast([128, 16, 64]),
                   ALU.mult, ["xs_f", "sm2"], ["xdtw"])
                TT(S_f[:].rearrange("p (h d) -> p h d", h=16), S_f[:].rearrange("p (h d) -> p h d", h=16),
                   cdec.unsqueeze(2).to_broadcast([128, 16, 64]), ALU.mult, ["S_f", "dec"], ["S_f"])
                for g in range(2):
                    b = mmc["n"] % 2
                    mmc["n"] += 1
                    MM(pAB[b][:], bc_b[:, g * 128:(g + 1) * 128], xdtw[:, g * 512:(g + 1) * 512], True, True,
                       ["bc_b", "xdtw"], ["pAB%d" % b])
                    TT(S_f[:, g * 512:(g + 1) * 512], S_f[:, g * 512:(g + 1) * 512], pAB[b][:], ALU.add,
                       ["S_f", "pAB%d" % b], ["S_f"])
                CP(S_b[:], S_f[:], ["S_f"], ["S_b"], eng="act")
                drain()

            def attention(n, cur, prev):
                for half in range(2):
                    for hh in range(8):
                        h = half * 8 + hh
                        TR(pT[0:64, hh * 128:(hh + 1) * 128], q_sb[:, h * 64:(h + 1) * 64], idb[:], ["q_sb", "idb"], ["pT"])
                    CP(QT[0:64, half * 8:(half + 1) * 8, :], pT[0:64, :].rearrange("p (a t) -> p a t", a=8), ["pT"], ["QT"],
                       eng=("act" if half else "dve"))
                kv_to_attn(cur)
                slots = (0, prev, cur)
                for kv in range(4):
                    for seg in range(3):
                        if n == 0 and seg < 2:
                            src = bias_c0[seg, kv]
                        else:
                            src = bias_gen[seg, kv]
                        P.dma("sync", biasb[seg][:], src, writes=["biasb%d" % seg])
                        MM(pBig[:, seg * 512:(seg + 1) * 512], KT[slots[seg]][0:64, kv, :],
                           QT[0:64, 4 * kv:4 * kv + 4, :].rearrange("p a t -> p (a t)"), True, True,
                           ["KT%d" % slots[seg], "QT"], ["pBig%d" % seg])
                        STT(lgt[seg][:], pBig[:, seg * 512:(seg + 1) * 512], 0.125, biasb[seg][:], ALU.mult, ALU.add,
                            ["pBig%d" % seg, "biasb%d" % seg], ["lgt%d" % seg])
                        ACT(PT[seg][:].rearrange("p a t -> p (a t)"), lgt[seg][:], AF.Exp, ["lgt%d" % seg], ["PT%d" % seg])
                    for hl in range(4):
                        for seg in range(3):
                            MM(pBig[:, 1536 + hl * 65:1536 + (hl + 1) * 65], PT[seg][:, hl, :], vext[slots[seg]][:, kv, :],
                               seg == 0, seg == 2, ["PT%d" % seg, "vext%d" % slots[seg]], ["pBig3"])
                    CP(ao[:, 4 * kv:4 * kv + 4, :].rearrange("p a d -> p (a d)"), pBig[:, 1536:1536 + 260], ["pBig3"], ["ao"],
                       eng="act")
                TT(sm[:, 3, :], ao[:, :, 64], esink[:], ALU.add, ["ao", "esink"], ["sm3"])
                RECIP(sm[:, 3, :], sm[:, 3, :], ["sm3"], ["sm3"])
                TT(scr[1][:].rearrange("p (h d) -> p h d", h=16), ao[:, :, 0:64],
                   sm[:, 3, :].unsqueeze(2).to_broadcast([128, 16, 64]), ALU.mult, ["ao", "sm3"], ["scr1"])
                rms_rstd(scr[1][:], ss, junk[:, 0:1024], 1024, ["scr1"], "m")
                TS(hn[:, 0:1024], scr[1][:], ss[:, 2:3], None, ALU.mult, None, ["scr1", "m_ss"], ["hn"])

            def out_proj(n):
                transpose16(hn, 1, "hn")
                for t in range(4):
                    wi = load_w(8 + t)
                    pt, pk = proj_tile(wi)
                    xb = cstate["xb"]
                    TT(scr[t % 2][:, 0:512], pt[:], x_t[xb][:, t * 512:(t + 1) * 512], ALU.add, [pk, "x_t%d" % xb],
                       ["scr%d" % (t % 2)])
                    P.dma("sync", h2s[n * 128:(n + 1) * 128, t * 512:(t + 1) * 512], scr[t % 2][:, 0:512],
                          reads=["scr%d" % (t % 2)], writes=["h2s%d" % n])

            for _ in range(NW):
                issue_w()
            issue_x(0)
            issue_x(1)
            norm_T(0)
            wi = load_w(2)
            pt, pk = proj_tile(wi)
            CP(kv_sb[:], pt[:], [pk], ["kv_sb"], eng="act")
            kv_to_attn(0)
            par = 0
            for ci in range(NPRE):
                chunk(ci, "pre_kv" if ci == NPRE - 1 else "pre", par)
                par = 1 - par
            kv_to_attn(1)
            prev = 1
            for n in range(NMAIN):
                cur = 3 - prev
                chunk(NPRE + n, "main", par)
                par = 1 - par
                attention(n, cur, prev)
                out_proj(n)
                prev = cur
            P.barrier()
            P.emit()

        with ExitStack() as st2:
            sb = lambda name, shape, dt: st2.enter_context(nc.sbuf_tensor(name, shape, dt))
            pa0 = st2.enter_context(nc.psum_tensor("pa0", [128, 512], F32))
            pacc = st2.enter_context(nc.psum_tensor("pacc", [128, 2048], F32))
            lnf = sb("lnf", [128, D], F32)
            acc = sb("acc", [128, 4, D], F32)
            xn2T = sb("xn2T", [128, 16, 512], BF16)
            ss2 = sb("ss2", [128, 8], F32)
            A1 = [sb("A1_%d" % t, [128, 8, 128], F32) for t in range(4)]
            S2 = [sb("S2_%d" % t, [128, 8, 128], F32) for t in range(4)]
            Dk = [sb("Dk_%d" % t, [128, 8, 128], BF16) for t in range(4)]
            NU = 3
            NV = 8

            P.dma("sync", lnf[:], lnfin, writes=["lnf"])

            for grp in range(2):
                with ExitStack() as st3:
                    pT2 = st3.enter_context(nc.psum_tensor("pT2_%d" % grp, [128, 1024], BF16))
                    pa1 = st3.enter_context(nc.psum_tensor("pa1_%d" % grp, [128, 512], F32))
                    pa = [pa0, pa1]
                    sb3 = lambda name, shape, dt: st3.enter_context(nc.sbuf_tensor(name + "_g%d" % grp, shape, dt))
                    keysb = sb3("keysb", [128, 16, 128], BF16)
                    junk2 = sb3("junk2", [128, D], BF16)
                    P.dma("pool", keysb[:], keysT, writes=["keysb"])
                    qTc = [sb3("qTc%d" % i, [128, 512], BF16) for i in range(2)]
                    scall = sb3("scall", [128, 4, 16, 128], F32)
                    topsall = sb3("topsall", [128, 4, 16, 16], F32)
                    xn2 = sb3("xn2", [128, D], BF16)
                    scw = [sb3("scw%d" % i, [128, 4, 128], F32) for i in range(2)]
                    cand = sb3("cand", [128, 8, 256], F32)
                    candw = sb3("candw", [128, 8, 256], F32)
                    ctop = sb3("ctop", [128, 8, 16], F32)
                    st8 = sb3("st8", [128, 6, 8], F32)
                    wqb = [sb3("wqb%d" % i, [128, 16, 128], BF16) for i in range(2)]
                    for tt in range(4):
                        n = grp * 4 + tt
                        P.dma("sync", acc[:, tt, :], h2s[n * 128:(n + 1) * 128, :], reads=["h2s%d" % n], writes=["acc%d" % tt])
                        rms_rstd(acc[:, tt, :], ss2, junk2[:], D, ["acc%d" % tt], "p")
                        TS(xn2[:], acc[:, tt, :], ss2[:, 2:3], None, ALU.mult, None, ["acc%d" % tt, "p_ss"], ["xn2"])
                        for half in range(2):
                            for kk in range(8):
                                k = half * 8 + kk
                                TR(pT2[:, kk * 128:(kk + 1) * 128], xn2[:, k * 128:(k + 1) * 128], idb[:], ["xn2", "idb"], ["pT2"])
                            for kk in range(8):
                                k = half * 8 + kk
                                TS(xn2T[:, k, tt * 128:(tt + 1) * 128], pT2[:, kk * 128:(kk + 1) * 128], nwt[:, 2, k:k + 1],
                                   None, ALU.mult, None, ["pT2", "nwt"], ["xn2T"])
                    for c in range(16):
                        wi = c % 2
                        P.dma("pool", wqb[wi][:], wq_blk[c], writes=["wqb%d" % wi])
                        for k in range(16):
                            MM(pa[wi][:], wqb[wi][:, k, :], xn2T[:, k, :], k == 0, k == 15, ["wqb%d" % wi, "xn2T"], ["pa%d" % wi])
                        CP(qTc[wi][:], pa[wi][:], ["pa%d" % wi], ["qTc%d" % wi], eng="act")
                        pb = c % 4
                        for tt in range(4):
                            MM(pacc[:, pb * 512 + tt * 128:pb * 512 + (tt + 1) * 128], qTc[wi][:, tt * 128:(tt + 1) * 128],
                               keysb[:, c, :], True, True, ["qTc%d" % wi, "keysb"], ["pacc%d" % pb])
                        CP(scall[:, :, c, :], pacc[:, pb * 512:(pb + 1) * 512].rearrange("p (a k) -> p a k", a=4),
                           ["pacc%d" % pb], ["sc_%d" % c], eng="act")
                        sw = c % 2
                        for tt in range(4):
                            OP("dve", lambda e, c=c, tt=tt: e.max(out=topsall[:, tt, c, 0:8], in_=scall[:, tt, c, :]),
                               ["sc_%d" % c], ["tA%d_%d" % (tt, c)])
                        for tt in range(4):
                            OP("dve", lambda e, c=c, tt=tt, sw=sw: e.match_replace(out=scw[sw][:, tt, :],
                                                                                   in_to_replace=topsall[:, tt, c, 0:8],
                                                                                   in_values=scall[:, tt, c, :], imm_value=NEG),
                               ["sc_%d" % c, "tA%d_%d" % (tt, c)], ["scw%d_%d" % (sw, tt)])
                        for tt in range(4):
                            OP("dve", lambda e, c=c, tt=tt, sw=sw: e.max(out=topsall[:, tt, c, 8:16], in_=scw[sw][:, tt, :]),
                               ["scw%d_%d" % (sw, tt)], ["tB%d_%d" % (tt, c)])
                    sck = ["sc_%d" % c for c in range(16)]
                    for tt in range(4):
                        tops = topsall[:, tt]
                        sc = scall[:, tt]
                        tk = ["tA%d_%d" % (tt, c) for c in range(16)] + ["tB%d_%d" % (tt, c) for c in range(16)]
                        t4 = tops.rearrange("p (h c) k -> p h c k", c=2)
                        TT(cand[:].rearrange("p h (a b) -> p h a b", a=16),
                           t4[:, :, 0, :].unsqueeze(3).to_broadcast([128, 8, 16, 16]),
                           t4[:, :, 1, :].unsqueeze(2).to_broadcast([128, 8, 16, 16]), ALU.add, tk, ["cand"])
                        for h in range(8):
                            OP("dve", lambda e, h=h: e.max(out=ctop[:, h, 0:8], in_=cand[:, h, :]), ["cand"], ["ctopA%d" % h])
                        for h in range(8):
                            OP("dve", lambda e, h=h: e.match_replace(out=candw[:, h, :], in_to_replace=ctop[:, h, 0:8],
                                                                       in_values=cand[:, h, :], imm_value=NEG),
                               ["cand", "ctopA%d" % h], ["candw%d" % h])
                        for h in range(8):
                            OP("dve", lambda e, h=h: e.max(out=ctop[:, h, 8:16], in_=candw[:, h, :]), ["candw%d" % h], ["ctopB%d" % h])
                        ck = ["ctopA%d" % h for h in range(8)] + ["ctopB%d" % h for h in range(8)]
                        TT(cand[:, :, 0:16], ctop[:], ctop[:, :, 0:1].to_broadcast([128, 8, 16]), ALU.subtract, ck, ["cand"])
                        ACT(cand[:, :, 0:16], cand[:, :, 0:16], AF.Exp, ["cand"], ["cand"])
                        OP("dve", lambda e: e.tensor_reduce(out=st8[:, 0, :], in_=cand[:, :, 0:16], axis=AX.X, op=ALU.add),
                           ["cand"], ["st8"])
                        RECIP(st8[:, 1, :], st8[:, 0, :], ["st8"], ["st8"])
                        TT(st8[:, 2, :], cand[:, :, 15], st8[:, 1, :], ALU.mult, ["cand", "st8"], ["st8"])
                        TS(st8[:, 3, :], ctop[:, :, 15], -DELTA, None, ALU.add, None, ck, ["st8"])
                        sc4 = sc.rearrange("p (h c) k -> p h c k", c=2)
                        TT(A1[tt][:], sc4[:, :, 0, :], st8[:, 3, :].unsqueeze(2).to_broadcast([128, 8, 128]), ALU.subtract,
                           sck + ["st8"], ["A1_%d" % tt])
                        CP(S2[tt][:], sc4[:, :, 1, :], sck, ["S2_%d" % tt])
                        TT(Dk[tt][:], ident_f.unsqueeze(1).to_broadcast([128, 8, 128]),
                           st8[:, 2, :].unsqueeze(2).to_broadcast([128, 8, 128]), ALU.mult, ["cmt", "st8"], ["Dk_%d" % tt])
                    P.barrier()
                    P.emit()
                with ExitStack() as st3:
                    pG = st3.enter_context(nc.psum_tensor("pG_%d" % grp, [128, 512], F32))
                    pGT = st3.enter_context(nc.psum_tensor("pGT_%d" % grp, [128, 2048], BF16))
                    sb3 = lambda name, shape, dt: st3.enter_context(nc.sbuf_tensor(name + "_g%d" % grp, shape, dt))
                    ub = [sb3("ub%d" % i, [128, 16, 128], BF16) for i in range(NU)]
                    vb = [sb3("vb%d" % i, [128, D], BF16) for i in range(NV)]
                    aTs = sb3("aTs", [128, IA, 512], F32)
                    gaT = [sb3("gaT%d" % i, [128, IA, 512], BF16) for i in range(2)]
                    NR = 3
                    cB = [sb3("cB%d" % i, [128, 8, 128], F32) for i in range(NR)]
                    Eb = [sb3("Eb%d" % i, [128, 8, 128], F32) for i in range(2)]
                    MEq = [sb3("MEq%d" % i, [128, IA, 8, 128], BF16) for i in range(2)]
                    Gs = [sb3("Gs%d" % i, [128, IA, 128], BF16) for i in range(2)]
                    wTs = [sb3("wTs%d" % i, [128, 512], BF16) for i in range(2 * IA)]
                    NBLK = 128 // IA
                    st = {"un": 0, "vn": 0, "en": 0, "ee": 0, "mq": 0, "gs": 0}

                    def u_mm(blk, il):
                        i = blk * IA + il
                        us = st["un"] % NU
                        st["un"] += 1
                        P.dma("pool", ub[us][:], u_blk[i], writes=["ub%d" % us])
                        for k in range(16):
                            MM(pa0[:], ub[us][:, k, :], xn2T[:, k, :], k == 0, k == 15, ["ub%d" % us, "xn2T"], ["pa0"])

                    def u_copy(il):
                        CP(aTs[:, il, :], pa0[:], ["pa0"], ["aTs%d" % il], eng="dve")

                    def gelu_stage(blk):
                        gq = blk % 2
                        ACT(gaT[gq][:].rearrange("p a t -> p (a t)"), aTs[:].rearrange("p a t -> p (a t)"), AF.Gelu,
                            ["aTs%d" % il for il in range(IA)], ["gaT%d" % gq])

                    def v_mm(blk, tt, vsl):
                        gq = blk % 2
                        for hq in range(2):
                            for il in range(IA):
                                ws = gq * IA + il
                                for dd in range(2):
                                    dq = hq * 2 + dd
                                    MM(pacc[:, dq * 512:(dq + 1) * 512], wTs[ws][:, tt * 128:(tt + 1) * 128],
                                       vb[vsl[il]][:, dq * 512:(dq + 1) * 512], il == 0, il == IA - 1,
                                       ["wTs%d" % ws, "vb%d" % vsl[il]], ["pacc%d" % dq])

                    def v_add(tt):
                        for hq in range(2):
                            TT(acc[:, tt, hq * 1024:(hq + 1) * 1024], acc[:, tt, hq * 1024:(hq + 1) * 1024],
                               pacc[:, hq * 1024:(hq + 1) * 1024], ALU.add,
                               ["acc%d_%d" % (tt, hq), "pacc%d" % (2 * hq), "pacc%d" % (2 * hq + 1)], ["acc%d_%d" % (tt, hq)])

                    def wT_stage(blk):
                        gq = blk % 2
                        for il in range(IA):
                            ws = gq * IA + il
                            TT(wTs[ws][:], gaT[gq][:, il, :], pGT[:, il * 512:(il + 1) * 512], ALU.mult,
                               ["gaT%d" % gq, "pGT%d" % il], ["wTs%d" % ws])

                    def gt_stage(pg):
                        tt_, gs_ = pg
                        CP(Gs[gs_][:].rearrange("p a j -> p (a j)"), pG[:], ["pG"], ["Gs%d" % gs_], eng="dve")
                        for il in range(IA):
                            TR(pGT[:, (il * 4 + tt_) * 128:(il * 4 + tt_ + 1) * 128], Gs[gs_][:, il, :], idb[:],
                               ["Gs%d" % gs_, "idb"], ["pGT%d" % il])

                    for il in range(IA):
                        u_mm(0, il)
                        u_copy(il)
                    gelu_stage(0)
                    prev_vsl = None
                    pend_add = None
                    pend_wT = None
                    pend_G = None
                    pend_gelu = None
                    for blk in range(NBLK):
                        vsl = []
                        for il in range(IA):
                            vs = st["vn"] % NV
                            st["vn"] += 1
                            vsl.append(vs)
                        for tt in range(4):
                            if blk + 1 < NBLK:
                                u_mm(blk + 1, tt)
                            mq = st["mq"] % 2
                            st["mq"] += 1
                            for il in range(IA):
                                i = blk * IA + il
                                eb = st["en"] % NR
                                st["en"] += 1
                                ee = st["ee"] % 2
                                st["ee"] += 1
                                TT(cB[eb][:], S2[tt][:], A1[tt][:, :, i:i + 1].to_broadcast([128, 8, 128]), ALU.add,
                                   ["S2_%d" % tt, "A1_%d" % tt], ["cB%d" % eb], eng="pool")
                                ACT(Eb[ee][:].rearrange("p h j -> p (h j)"), cB[eb][:].rearrange("p h j -> p (h j)"), AF.Prelu,
                                    ["cB%d" % eb], ["Eb%d" % ee], alpha=1e10)
                                ACT(MEq[mq][:, il, :, :].rearrange("p h j -> p (h j)"), Eb[ee][:].rearrange("p h j -> p (h j)"), AF.Exp,
                                    ["Eb%d" % ee], ["MEq%d" % mq])
                            P.dma("pool", vb[vsl[tt]][:], v_blk[blk * IA + tt], writes=["vb%d" % vsl[tt]])
                            if pend_G is not None:
                                gt_stage(pend_G)
                                pend_G = None
                            if pend_add is not None:
                                v_add(pend_add)
                                pend_add = None
                            if pend_wT is not None:
                                wT_stage(pend_wT)
                                pend_wT = None
                            if pend_gelu is not None:
                                gelu_stage(pend_gelu)
                                pend_gelu = None
                            if blk + 1 < NBLK:
                                u_copy(tt)
                            for h in range(8):
                                MM(pG[:], Dk[tt][:, h, :], MEq[mq][:, :, h, :], h == 0, h == 7,
                                   ["Dk_%d" % tt, "MEq%d" % mq], ["pG"])
                            gs = st["gs"] % 2
                            st["gs"] += 1
                            pend_G = (tt, gs)
                            if blk > 0:
                                v_mm(blk - 1, tt, prev_vsl)
                                pend_add = tt
                        pend_wT = blk
                        if blk + 1 < NBLK:
                            pend_gelu = blk + 1
                        prev_vsl = vsl
                    if pend_add is not None:
                        v_add(pend_add)
                    gt_stage(pend_G)
                    wT_stage(NBLK - 1)
                    for tt in range(4):
                        v_mm(NBLK - 1, tt, prev_vsl)
                        v_add(tt)
                    jk = MEq[0][:].rearrange("p a h j -> p (a h j)")[:, 0:D]
                    for tt in range(4):
                        n = grp * 4 + tt
                        ak = ["acc%d_0" % tt, "acc%d_1" % tt]
                        rms_rstd(acc[:, tt, :], ss2, jk, D, ak, "p", jkey="MEq0")
                        STT(acc[:, tt, :], acc[:, tt, :], ss2[:, 2:3], lnf[:], ALU.mult, ALU.mult, ak + ["p_ss", "lnf"], ak)
                        P.dma("sync", out[n * 128:(n + 1) * 128, :], acc[:, tt, :], reads=ak, writes=["out%d" % n])
                    P.barrier()
                    P.emit()
    return nc


def _t5_bucket(dist):
    n = np.maximum(dist, 0)
    max_exact = 16
    large = max_exact + (np.log(np.maximum(n, 1) / max_exact) / np.log(128 / max_exact) * (32 - max_exact)).astype(np.int32)
    large = np.minimum(large, 31)
    return np.where(n < max_exact, n, large).astype(np.int32)


def _bias_tables(rel_bias, blk):
    qi = np.arange(128)[None, :]
    kj = np.arange(128)[:, None]
    out = np.full((3, 16, 128, 128), NEG, np.float32)
    rb = rel_bias.astype(np.float32)
    m = np.arange(16)[:, None]
    dist = 16 + blk * 128 + qi - m
    out[0, :, 112:128, :] = np.transpose(rb[_t5_bucket(dist)], (2, 0, 1))
    if blk > 0:
        dist = 128 + qi - kj
        valid = (dist < 128)
        g = np.transpose(rb[_t5_bucket(dist)], (2, 0, 1))
        out[1] = np.where(valid[None], g, NEG)
    dist = qi - kj
    valid = dist >= 0
    g = np.transpose(rb[_t5_bucket(dist)], (2, 0, 1))
    out[2] = np.where(valid[None], g, NEG)
    o = out.reshape(3, 4, 4, 128, 128).transpose(0, 1, 3, 2, 4).reshape(3, 4, 128, 512)
    return np.ascontiguousarray(o)


_NC_CACHE = {}


def kernel(x, meta_tokens, rel_bias, ln_mix, w_in, attn_sinks, conv_w, conv_b, dt_bias, a_log, d_skip,
           attn_norm_w, ssm_norm_w, w_out, ln_ffn, peer_wq, peer_keys, peer_u, peer_v, ln_final):
    f = lambda a: np.asarray(a, dtype=np.float32)
    x = f(x); meta = f(meta_tokens); rel_bias = f(rel_bias)
    w_in = f(w_in)[0]; w_out = f(w_out)[0]; wq = f(peer_wq)[0]
    keys = f(peer_keys)[0]; u = f(peer_u)[0]; v = f(peer_v)[0]
    rep = lambda a, shape: np.ascontiguousarray(np.broadcast_to(a, shape)).astype(np.float32)
    blkw = lambda w, nt, cw: np.ascontiguousarray(w.reshape(16, 128, nt, cw).transpose(2, 1, 0, 3))
    w_in_blk = blkw(w_in[:, :4096], 8, 512)
    w_dt = np.ascontiguousarray(w_in[:, 4096:4112].reshape(16, 128, 16).transpose(1, 0, 2))
    w_out_blk = blkw(w_out, 4, 512)
    wq_blk = blkw(wq, 16, 128)
    keysT = np.ascontiguousarray(keys.reshape(16, 128, 128).transpose(2, 0, 1))
    u_blk = np.ascontiguousarray(u.reshape(128, 128, 16, 128).transpose(0, 3, 2, 1))
    v_blk = v.reshape(128, 128, D)
    convw_r = rep(f(conv_w)[0][None], (128, 4, 1536))
    convb_r = f(conv_b)[0][None, :]
    hv = rep(np.stack([f(dt_bias)[0], f(a_log)[0], f(d_skip)[0], f(attn_sinks)[0]])[None], (128, 4, 16))
    tl = lambda w: w.reshape(16, 128).T
    nwa = np.ascontiguousarray(np.stack([tl(f(ln_mix)[0]), tl(np.concatenate([f(attn_norm_w)[0], f(ssm_norm_w)[0]])),
                                         tl(f(ln_ffn)[0])], axis=1)).astype(np.float32)
    lnfin = rep(f(ln_final)[None], (128, D))
    ar = np.arange(128)
    cmat = np.stack([np.eye(128), (ar[:, None] <= ar[None, :]), (ar[:, None] > ar[None, :]), np.ones((128, 128))],
                    axis=1).astype(np.float32)
    sh = np.zeros((128, 7, 128), np.float32)
    for j in range(4):
        s = 3 - j
        sh[:, j, :] = (ar[:, None] == ar[None, :] - s)
    for j in range(3):
        s = 3 - j
        sh[:, 4 + j, :] = (ar[:, None] == 128 + ar[None, :] - s)
    bias_gen = _bias_tables(rel_bias, 1)
    bias_first = _bias_tables(rel_bias, 0)
    common = dict(w_in_blk=w_in_blk, w_dt=w_dt, w_out_blk=w_out_blk, wq_blk=wq_blk, keysT=keysT, u_blk=u_blk,
                  v_blk=v_blk, convw_r=convw_r, convb_r=convb_r, hv=hv, nw=nwa, lnfin=lnfin, cm=cmat, shm_r=sh,
                  bias_gen=bias_gen)
    xmeta = np.zeros((128, D), np.float32)
    xmeta[112:] = meta
    in_maps = []
    for c in range(NCORES):
        b, half = c // 2, c % 2
        xt = np.zeros(((NPRE + NMAIN) * 128, D), np.float32)
        pm = np.zeros((128, NPRE), np.float32)
        if half == 1:
            xt[112:128] = meta
            xt[128:] = x[b]
            pm[112:, 0] = 1.0
            pm[:, 1:] = 1.0
            bc0 = bias_gen[0:2]
        else:
            xt[NPRE * 128 - 16:NPRE * 128] = meta
            xt[NPRE * 128:] = x[b, :1024]
            pm[112:, NPRE - 1] = 1.0
            bc0 = bias_first[0:2]
        d = dict(common)
        d.update(xtok=xt, xmeta=xmeta, padmask=pm, bias_c0=np.ascontiguousarray(bc0))
        in_maps.append(d)
    if "nc" not in _NC_CACHE:
        _NC_CACHE["nc"] = build_nc()
    res = run_bass_kernel_spmd(_NC_CACHE["nc"], in_maps, core_ids=list(range(NCORES)))
    outp = np.empty((4, 2048, D), np.float32)
    for c in range(NCORES):
        b, half = c // 2, c % 2
        outp[b, half * 1024:(half + 1) * 1024] = res.results[c]["out"]
    return outp
```

```python
import numpy as np
from contextlib import ExitStack
import concourse.bass as bass
import concourse.mybir as mybir
from concourse.bass_utils import run_bass_kernel_spmd

F32 = mybir.dt.float32
BF16 = mybir.dt.bfloat16
AF = mybir.ActivationFunctionType
ALU = mybir.AluOpType
AX = mybir.AxisListType

SAME_ENGINE_SYNC = True
NDMA_SEM = 6
NCORES = 8
D = 2048
NPRE = 9
NMAIN = 8
EPS = 1e-6
NEG = -1e30
DELTA = 1e-5
IA = 4
NW = 4


class Prog:
    CMP = ("pe", "act", "dve", "pool")

    def __init__(self, nc):
        self.nc = nc
        self.streams = {e: [] for e in ("pe", "act", "dve", "pool", "sync")}
        self.cnt = {e: 0 for e in self.CMP}
        self.sem = {}
        self.dma_sems = {}
        self.dma_n = {}
        self.waited = {e: {} for e in self.streams}
        self.last_w = {}
        self.readers = {}

    def setup(self, stack):
        nc = self.nc
        for e in self.CMP:
            self.sem[e] = stack.enter_context(nc.semaphore("c_" + e))
        for q in ("sync", "act", "pool"):
            self.dma_sems[q] = [stack.enter_context(nc.semaphore("d_%s%d" % (q, i))) for i in range(NDMA_SEM)]
            self.dma_n[q] = 0

    def _deps(self, reads, writes):
        toks = []
        for k in reads:
            t = self.last_w.get(k)
            if t is not None:
                toks.append(t + (0,))
        for k in writes:
            t = self.last_w.get(k)
            if t is not None:
                toks.append(t + (1,))
            for t in self.readers.get(k, ()):
                toks.append(t + (1,))
        return toks

    def _commit(self, tok, reads, writes):
        for k in reads:
            self.readers.setdefault(k, []).append(tok)
        for k in writes:
            self.last_w[k] = tok
            self.readers[k] = []

    def _waits(self, stream, toks, is_dma=False):
        need = {}
        for (sk, val, src, kind) in toks:
            if (not is_dma) and src == stream:
                if stream == "pe" or not SAME_ENGINE_SYNC or kind == 1:
                    continue
            if need.get(sk, 0) < val:
                need[sk] = val
        out = []
        w = self.waited[stream]
        for sk, val in need.items():
            if w.get(sk, 0) >= val:
                continue
            w[sk] = val
            out.append((sk, val))
        return out

    def _semh(self, sk):
        if isinstance(sk, str):
            return self.sem[sk]
        return self.dma_sems[sk[0]][sk[1]]

    def op(self, eng, fn, reads=(), writes=(), inc=True):
        toks = self._deps(reads, writes)
        waits = self._waits(eng, toks)
        if inc:
            self.cnt[eng] += 1
            tok = (eng, self.cnt[eng], eng)
            self.streams[eng].append((waits, fn, (eng, 1)))
        else:
            tok = (eng, self.cnt[eng] + 1, eng)
            self.streams[eng].append((waits, fn, None))
        self._commit(tok, reads, writes)

    def dma(self, q, out, in_, reads=(), writes=()):
        toks = self._deps(reads, writes)
        n = self.dma_n[q]
        self.dma_n[q] += 1
        si = n % NDMA_SEM
        rnd = n // NDMA_SEM
        sk = (q, si)
        if rnd > 0:
            toks.append((sk, 16 * rnd, "dma", 1))
        waits = self._waits(q, toks, is_dma=True)
        tok = (sk, 16 * (rnd + 1), "dma")

        def fn(e, out=out, in_=in_):
            return e.dma_start(out=out, in_=in_)

        self.streams[q].append((waits, fn, (sk, 16)))
        self._commit(tok, reads, writes)

    def _all_tokens(self):
        need = []
        for q in self.dma_sems:
            n = self.dma_n[q]
            for si in range(NDMA_SEM):
                cnt = (n - si + NDMA_SEM - 1) // NDMA_SEM if n > si else 0
                if cnt > 0:
                    need.append(((q, si), 16 * cnt))
        for e in self.CMP:
            if self.cnt[e] > 0:
                need.append((e, self.cnt[e]))
        return need

    def barrier(self):
        need = self._all_tokens()
        for s in self.streams:
            w = self.waited[s]
            ws = []
            for sk, val in need:
                if w.get(sk, 0) >= val:
                    continue
                w[sk] = val
                ws.append((sk, val))
            if ws:
                self.streams[s].append((ws, None, None))
        self.last_w = {}
        self.readers = {}

    def emit(self):
        nc = self.nc
        prog = self
        streams = self.streams
        self.streams = {e: [] for e in streams}
        with nc.Block() as block:
            def run(stream, e):
                for (waits, fn, inc) in streams[stream]:
                    for (sk, val) in waits:
                        e.wait_ge(prog._semh(sk), val)
                    if fn is None:
                        continue
                    ins = fn(e)
                    if inc is not None:
                        ins.then_inc(prog._semh(inc[0]), inc[1])

            @block.tensor
            def _(e):
                run("pe", e)

            @block.scalar
            def _(e):
                run("act", e)

            @block.vector
            def _(e):
                run("dve", e)

            @block.gpsimd
            def _(e):
                run("pool", e)

            @block.sync
            def _(e):
                run("sync", e)


def build_nc():
    nc = bass.Bass("TRN2", target_bir_lowering=False)
    din = lambda name, shape: nc.dram_tensor(name, list(shape), F32, kind="ExternalInput").ap()
    xtok = din("xtok", [(NPRE + NMAIN) * 128, D])
    xmeta = din("xmeta", [128, D])
    padmask = din("padmask", [128, NPRE])
    w_in_blk = din("w_in_blk", [8, 128, 16, 512])
    w_dt = din("w_dt", [128, 16, 16])
    w_out_blk = din("w_out_blk", [4, 128, 16, 512])
    wq_blk = din("wq_blk", [16, 128, 16, 128])
    keysT = din("keysT", [128, 16, 128])
    u_blk = din("u_blk", [128, 128, 16, 128])
    v_blk = din("v_blk", [128, 128, D])
    convw_r = din("convw_r", [128, 4, 1536])
    convb_r = din("convb_r", [1, 1536])
    hv = din("hv", [128, 4, 16])
    nw = din("nw", [128, 3, 16])
    lnfin = din("lnfin", [128, D])
    cm = din("cm", [128, 4, 128])
    shm_r = din("shm_r", [128, 7, 128])
    bias_gen = din("bias_gen", [3, 4, 128, 512])
    bias_c0 = din("bias_c0", [2, 4, 128, 512])
    out = nc.dram_tensor("out", [NMAIN * 128, D], F32, kind="ExternalOutput").ap()
    h2s = nc.dram_tensor("h2s", [NMAIN * 128, D], F32, kind="Internal").ap()
    wsc = nc.dram_tensor("wsc", [12, 128, 16, 512], BF16, kind="Internal").ap()

    with ExitStack() as st0:
        P = Prog(nc)
        P.setup(st0)

        def OP(eng, fn, r=(), w=()):
            P.op(eng, fn, reads=r, writes=w)

        def MM(out, lhsT, rhs, start, stop, r, w):
            P.op("pe", lambda e: e.matmul(out=out, lhsT=lhsT, rhs=rhs, start=start, stop=stop), reads=r, writes=w,
                 inc=bool(stop))

        def TR(out, in_, ident, r, w):
            P.op("pe", lambda e: e.transpose(out=out, in_=in_, identity=ident), reads=r, writes=w)

        def ACT(out, in_, func, r, w, bias=None, scale=None, accum=None, alpha=None):
            kw = {}
            if alpha is not None:
                kw["alpha"] = alpha
            if bias is not None:
                kw["bias"] = bias
            if scale is not None:
                kw["scale"] = scale
            if accum is not None:
                kw["accum_out"] = accum
            P.op("act", lambda e: e.activation(out=out, in_=in_, func=func, **kw), reads=r, writes=w)

        def TT(out, in0, in1, op, r, w, eng="dve"):
            P.op(eng, lambda e: e.tensor_tensor(out=out, in0=in0, in1=in1, op=op), reads=r, writes=w)

        def TS(out, in0, s1, s2, op0, op1, r, w, eng="dve"):
            if s2 is None:
                P.op(eng, lambda e: e.tensor_scalar(out=out, in0=in0, scalar1=s1, scalar2=None, op0=op0),
                     reads=r, writes=w)
            else:
                P.op(eng, lambda e: e.tensor_scalar(out=out, in0=in0, scalar1=s1, scalar2=s2, op0=op0, op1=op1),
                     reads=r, writes=w)

        def STT(out, in0, scalar, in1, op0, op1, r, w, eng="dve"):
            P.op(eng, lambda e: e.scalar_tensor_tensor(out=out, in0=in0, scalar=scalar, in1=in1, op0=op0, op1=op1),
                 reads=r, writes=w)

        def CP(out, in_, r, w, eng="dve"):
            if eng == "act":
                P.op("act", lambda e: e.copy(out=out, in_=in_), reads=r, writes=w)
            else:
                P.op(eng, lambda e: e.tensor_copy(out=out, in_=in_), reads=r, writes=w)

        def RECIP(out, in_, r, w):
            P.op("dve", lambda e: e.reciprocal(out=out, in_=in_), reads=r, writes=w)

        sbp = lambda name, shape, dt: st0.enter_context(nc.sbuf_tensor(name, shape, dt))
        cmt = sbp("cmt", [128, 4, 128], F32)
        idb = sbp("idb", [128, 128], BF16)
        nwt = sbp("nwt", [128, 3, 16], F32)
        ident_f = cmt[:, 0, :]
        triU = cmt[:, 1, :]
        SLm = cmt[:, 2, :]
        ones_f = cmt[:, 3, :]
        P.dma("sync", cmt[:], cm, writes=["cmt"])
        P.dma("sync", nwt[:], nw, writes=["nwt"])
        CP(idb[:], cmt[:, 0, :], ["cmt"], ["idb"])

        def rms_rstd(xin, ss, junk, n, r, key, jkey=None):
            ACT(junk, xin, AF.Square, r, [jkey or (key + "_junk"), key + "_ss"], accum=ss[:, 0:1])
            ACT(ss[:, 1:2], ss[:, 0:1], AF.Ln, [key + "_ss"], [key + "_ss"], bias=EPS, scale=1.0 / n)
            ACT(ss[:, 2:3], ss[:, 1:2], AF.Exp, [key + "_ss"], [key + "_ss"], scale=-0.5)

        with ExitStack() as st1:
            sb = lambda name, shape, dt: st1.enter_context(nc.sbuf_tensor(name, shape, dt))
            pA = st1.enter_context(nc.psum_tensor("pA", [128, 512], F32))
            pB = st1.enter_context(nc.psum_tensor("pB", [128, 512], F32))
            pS = st1.enter_context(nc.psum_tensor("pS", [128, 512], F32))
            pT = st1.enter_context(nc.psum_tensor("pT", [128, 1024], BF16))
            pBig = st1.enter_context(nc.psum_tensor("pBig", [128, 2048], F32))
            pAB = [pA, pB]

            x_t = [sb("x_t%d" % i, [128, D], F32) for i in range(2)]
            junk = sb("junk", [128, D], BF16)
            ss = sb("ss", [128, 8], F32)
            hn = sb("hn", [128, D], BF16)
            tT = sb("tT", [128, 16, 128], BF16)
            wbuf = [sb("wbuf%d" % i, [128, 16, 512], BF16) for i in range(NW)]
            wdt = sb("wdt", [128, 16, 16], BF16)
            q_sb = sb("q_sb", [128, 1024], BF16)
            kv_sb = sb("kv_sb", [128, 512], BF16)
            xw = [[sb("xw%d_%d" % (pp, j), [128, 1536], BF16) for j in range(4)] for pp in range(2)]
            convw = sb("convw", [128, 4, 1536], BF16)
            convb = sb("convb", [1, 1536], BF16)
            ones_row = sb("ones_row", [1, 128], BF16)
            shm = sb("shm", [128, 7, 128], BF16)
            hvt = sb("hvt", [128, 4, 16], F32)
            pmask = sb("pmask", [128, NPRE], F32)
            xs_f = sb("xs_f", [128, 1024], F32)
            bc_b = sb("bc_b", [128, 512], BF16)
            zs = sb("zs", [128, 1024], BF16)
            sm = sb("sm", [128, 8, 16], F32)
            dec = sb("dec", [128, 48], F32)
            dAU = [sb("dAU%d" % i, [128, 4, 128], F32) for i in range(2)]
            LT = sb("LT", [128, 16, 128], BF16)
            bcT = sb("bcT", [128, 4, 128], BF16)
            cbm = sb("cbm", [128, 2, 128], BF16)
            xdt = sb("xdt", [128, 1024], BF16)
            xdtw = sb("xdtw", [128, 1024], BF16)
            scr = [sb("scr%d" % i, [128, 1024], F32) for i in range(2)]
            S_f = sb("S_f", [128, 1024], F32)
            S_b = sb("S_b", [128, 1024], BF16)
            QT = sb("QT", [128, 16, 128], BF16)
            KT = [sb("KT%d" % i, [128, 4, 128], BF16) for i in range(3)]
            vext = [sb("vext%d" % i, [128, 4, 65], BF16) for i in range(3)]
            biasb = [sb("biasb%d" % i, [128, 512], F32) for i in range(3)]
            lgt = [sb("lgt%d" % i, [128, 512], F32) for i in range(3)]
            PT = [sb("PT%d" % i, [128, 4, 128], BF16) for i in range(3)]
            ao = sb("ao", [128, 16, 65], F32)
            esink = sb("esink", [128, 16], F32)
            Aneg = sb("Aneg", [128, 16], F32)

            P.dma("pool", convw[:], convw_r, writes=["convw"])
            P.dma("pool", convb[:], convb_r, writes=["convb"])
            P.dma("pool", shm[:], shm_r, writes=["shm"])
            P.dma("sync", hvt[:], hv, writes=["hvt"])
            P.dma("sync", pmask[:], padmask, writes=["pmask"])
            OP("dve", lambda e: e.memset(ones_row[:], 1.0), [], ["ones_row"])
            for i in range(3):
                OP("dve", lambda e, i=i: e.memset(vext[i][:, :, 64:65], 1.0), [], ["vext%d" % i])
            OP("dve", lambda e: e.memset(S_f[:], 0.0), [], ["S_f"])
            OP("dve", lambda e: e.memset(S_b[:], 0.0), [], ["S_b"])
            for j in range(4):
                OP("dve", lambda e, j=j: e.memset(xw[1][j][:], 0.0), [], ["xw1_%d" % j])
            ACT(Aneg[:], hvt[:, 1, :], AF.Exp, ["hvt"], ["Aneg"])
            TS(Aneg[:], Aneg[:], -1.0, None, ALU.mult, None, ["Aneg"], ["Aneg"])
            ACT(esink[:], hvt[:, 3, :], AF.Exp, ["hvt"], ["esink"])
            dtb = hvt[:, 0, :]
            dskip = hvt[:, 2, :]

            wseq = [2] + [5, 6, 7] * (NPRE - 1) + [5, 6, 7, 2] + [5, 6, 7, 3, 4, 0, 1, 2, 8, 9, 10, 11] * NMAIN
            wstate = {"use": 0, "iss": 0, "seen": set()}

            def issue_w():
                k = wstate["iss"]
                if k >= len(wseq):
                    return
                wstate["iss"] += 1
                tid = wseq[k]
                i = k % NW
                if tid not in wstate["seen"]:
                    wstate["seen"].add(tid)
                    src = w_in_blk[tid] if tid < 8 else w_out_blk[tid - 8]
                    P.dma("pool", wbuf[i][:], src, writes=["wbuf%d" % i])
                    P.dma("sync", wsc[tid], wbuf[i][:], reads=["wbuf%d" % i], writes=["wsc%d" % tid])
                else:
                    P.dma("sync", wbuf[i][:], wsc[tid], reads=["wsc%d" % tid], writes=["wbuf%d" % i])

            def load_w(tid):
                k = wstate["use"]
                assert wseq[k] == tid, (k, wseq[k], tid)
                wstate["use"] += 1
                return k % NW

            xseq = [xmeta] + [xtok[ci * 128:(ci + 1) * 128, :] for ci in range(NPRE + NMAIN)]
            xstate = {"n": 0}

            def issue_x(k):
                if k < len(xseq):
                    P.dma("sync", x_t[k % 2][:], xseq[k], writes=["x_t%d" % (k % 2)])

            def norm_T(nwi):
                k = xstate["n"]
                xstate["n"] += 1
                xb = k % 2
                rms_rstd(x_t[xb][:], ss, junk[:], D, ["x_t%d" % xb], "m")
                TS(hn[:], x_t[xb][:], ss[:, 2:3], None, ALU.mult, None, ["x_t%d" % xb, "m_ss"], ["hn"])
                transpose16(hn, nwi, "hn")
                return xb

            def transpose16(src, nwi, key):
                for half in range(2):
                    for kk in range(8):
                        k = half * 8 + kk
                        TR(pT[:, kk * 128:(kk + 1) * 128], src[:, k * 128:(k + 1) * 128], idb[:], [key, "idb"], ["pT"])
                    for kk in range(8):
                        k = half * 8 + kk
                        TS(tT[:, k, :], pT[:, kk * 128:(kk + 1) * 128], nwt[:, nwi, k:k + 1], None, ALU.mult, None,
                           ["pT", "nwt"], ["tT"], eng=("dve" if kk % 2 == 0 else "dve"))

            mmc = {"n": 0}

            def proj_tile(wi, ncols=512):
                b = mmc["n"] % 2
                mmc["n"] += 1
                pk = "pAB%d" % b
                for k in range(16):
                    MM(pAB[b][:, 0:ncols], tT[:, k, :], wbuf[wi][:, k, 0:ncols], k == 0, k == 15,
                       ["tT", "wbuf%d" % wi], [pk])
                issue_w()
                return pAB[b], pk

            def kv_to_attn(slot):
                for kv in range(4):
                    TR(pT[0:64, kv * 128:(kv + 1) * 128], kv_sb[:, kv * 64:(kv + 1) * 64], idb[:], ["kv_sb", "idb"], ["pT"])
                CP(KT[slot][0:64, :, :], pT[0:64, 0:512].rearrange("p (a t) -> p a t", a=4), ["pT"], ["KT%d" % slot])
                CP(vext[slot][:, :, 0:64], kv_sb[:, 256:512].rearrange("p (a d) -> p a d", a=4), ["kv_sb"],
                   ["vext%d" % slot], eng="act")

            cstate = {}

            def chunk(ci, mode, par):
                main = mode == "main"
                xb = norm_T(0)
                cstate["xb"] = xb
                for tix, c0 in ((5, 0), (6, 512), (7, 1024)):
                    wi = load_w(tix)
                    pt, pk = proj_tile(wi)
                    for j in range(4):
                        TT(xw[par][j][:, c0:c0 + 512], pt[:], convw[:, j, c0:c0 + 512], ALU.mult,
                           [pk, "convw"], ["xw%d_%d" % (par, j)])
                P.dma("pool", wdt[:], w_dt, writes=["wdt"]) if ci == 0 else None
                for k in range(16):
                    MM(pS[:, 0:16], tT[:, k, :], wdt[:, k, :], k == 0, k == 15, ["tT", "wdt"], ["pS"])
                TT(sm[:, 0, :], pS[:, 0:16], dtb, ALU.add, ["pS", "hvt"], ["sm0"])
                ACT(sm[:, 0, :], sm[:, 0, :], AF.Exp, ["sm0"], ["sm0"])
                ACT(sm[:, 0, :], sm[:, 0, :], AF.Ln, ["sm0"], ["sm0"], bias=1.0)
                if not main:
                    TS(sm[:, 0, :], sm[:, 0, :], pmask[:, ci:ci + 1], None, ALU.mult, None, ["sm0", "pmask"], ["sm0"])
                dt = sm[:, 0, :]
                TT(sm[:, 1, :], dt, Aneg[:], ALU.mult, ["sm0", "Aneg"], ["sm1"])
                dA = sm[:, 1, :]
                def rest_tiles():
                    if main:
                        for tix, c0 in ((3, 0), (4, 512)):
                            wi = load_w(tix)
                            pt, pk = proj_tile(wi)
                            ACT(zs[:, c0:c0 + 512], pt[:], AF.Silu, [pk], ["zs"])
                            yield
                        for tix, c0 in ((0, 0), (1, 512)):
                            wi = load_w(tix)
                            pt, pk = proj_tile(wi)
                            CP(q_sb[:, c0:c0 + 512], pt[:], [pk], ["q_sb"], eng="act")
                            yield
                    if main or mode == "pre_kv":
                        wi = load_w(2)
                        pt, pk = proj_tile(wi)
                        CP(kv_sb[:], pt[:], [pk], ["kv_sb"], eng="act")
                        yield

                rest = rest_tiles()

                def adv():
                    next(rest, None)

                def drain():
                    for _ in rest:
                        pass

                issue_x(xstate["n"])
                for cb in range(3):
                    c0 = cb * 512
                    pk = "pBig%d" % cb
                    o = pBig[:, c0:c0 + 512]
                    for j in range(4):
                        MM(o, shm[:, j, :], xw[par][j][:, c0:c0 + 512], j == 0, False, ["shm", "xw%d_%d" % (par, j)], [pk])
                    for j in range(3):
                        MM(o, shm[:, 4 + j, :], xw[1 - par][j][:, c0:c0 + 512], False, False,
                           ["shm", "xw%d_%d" % (1 - par, j)], [pk])
                    MM(o, ones_row[0:1, :], convb[0:1, c0:c0 + 512], False, True, ["ones_row", "convb"], [pk])
                adv()
                ACT(xs_f[:, 0:512], pBig[:, 0:512], AF.Silu, ["pBig0"], ["xs_f"])
                ACT(xs_f[:, 512:1024], pBig[:, 512:1024], AF.Silu, ["pBig1"], ["xs_f"])
                ACT(bc_b[:], pBig[:, 1024:1536], AF.Silu, ["pBig2"], ["bc_b"])
                MM(pS[:, 16:32], SLm, dA, True, True, ["cmt", "sm1"], ["pS"])
                MM(pS[:, 32:48], ones_f, dA, True, True, ["cmt", "sm1"], ["pS"])
                MM(pS[:, 48:64], triU, dA, True, True, ["cmt", "sm1"], ["pS"])
                adv()
                ACT(dec[:], pS[:, 16:64], AF.Exp, ["pS"], ["dec"])
                Lend, cdec, ecs = dec[:, 0:16], dec[:, 16:32], dec[:, 32:48]
                xs3 = xs_f[:].rearrange("p (h d) -> p h d", h=16)
                if main:
                    for qd in range(4):
                        bi = qd % 2
                        TT(dAU[bi][:], dA[:, qd * 4:(qd + 1) * 4].unsqueeze(2).to_broadcast([128, 4, 128]),
                           triU.unsqueeze(1).to_broadcast([128, 4, 128]), ALU.mult, ["sm1", "cmt"], ["dAU%d" % bi])
                        MM(pBig[:, qd * 512:(qd + 1) * 512], SLm, dAU[bi][:].rearrange("p a l -> p (a l)"), True, True,
                           ["cmt", "dAU%d" % bi], ["pBig%d" % qd])
                        adv()
                        ACT(LT[:, qd * 4:(qd + 1) * 4, :].rearrange("p a l -> p (a l)"), pBig[:, qd * 512:(qd + 1) * 512],
                            AF.Exp, ["pBig%d" % qd], ["LT"])
                    for a in range(4):
                        TR(pT[:, a * 128:(a + 1) * 128], bc_b[:, a * 128:(a + 1) * 128], idb[:], ["bc_b", "idb"], ["pT"])
                    CP(bcT[:].rearrange("p a t -> p (a t)"), pT[:, 0:512], ["pT"], ["bcT"])
                    bb = mmc["n"] % 2
                    mmc["n"] += 1
                    for g in range(2):
                        MM(pAB[bb][:, g * 128:(g + 1) * 128], bcT[:, g, :], bcT[:, 2 + g, :], True, True, ["bcT"], ["pAB%d" % bb])
                    TT(cbm[:], pAB[bb][:, 0:256].rearrange("p (g l) -> p g l", g=2),
                       triU.unsqueeze(1).to_broadcast([128, 2, 128]), ALU.mult, ["pAB%d" % bb, "cmt"], ["cbm"])
                    for g in range(2):
                        TT(LT[:, 8 * g:8 * g + 8, :], LT[:, 8 * g:8 * g + 8, :],
                           cbm[:, g, :].unsqueeze(1).to_broadcast([128, 8, 128]), ALU.mult, ["LT", "cbm"], ["LT"])
                    TT(xdt[:].rearrange("p (h d) -> p h d", h=16), xs3, dt.unsqueeze(2).to_broadcast([128, 16, 64]),
                       ALU.mult, ["xs_f", "sm0"], ["xdt"])
                    for h in range(16):
                        MM(pBig[:, h * 64:(h + 1) * 64], LT[:, h, :], xdt[:, h * 64:(h + 1) * 64], True, True,
                           ["LT", "xdt"], ["pBig%d" % (h // 8)])
                    for g in range(2):
                        MM(pBig[:, 1024 + g * 512:1024 + (g + 1) * 512], bcT[:, 2 + g, :], S_b[:, g * 512:(g + 1) * 512],
                           True, True, ["bcT", "S_b"], ["pBig%d" % (2 + g)])
                    CP(scr[0][:], pBig[:, 1024:2048], ["pBig2", "pBig3"], ["scr0"], eng="act")
                    s03 = scr[0][:].rearrange("p (h d) -> p h d", h=16)
                    s13 = scr[1][:].rearrange("p (h d) -> p h d", h=16)
                    TT(s03, s03, ecs.unsqueeze(2).to_broadcast([128, 16, 64]), ALU.mult, ["scr0", "dec"], ["scr0"])
                    TT(scr[0][:], scr[0][:], pBig[:, 0:1024], ALU.add, ["scr0", "pBig0", "pBig1"], ["scr0"])
                    TT(s13, xs3, dskip.unsqueeze(2).to_broadcast([128, 16, 64]), ALU.mult, ["xs_f", "hvt"], ["scr1"])
                    TT(scr[0][:], scr[0][:], scr[1][:], ALU.add, ["scr0", "scr1"], ["scr0"])
                    drain()
                    TT(scr[0][:], scr[0][:], zs[:], ALU.mult, ["scr0", "zs"], ["scr0"])
                    for g in range(2):
                        ACT(junk[:, 0:512], scr[0][:, g * 512:(g + 1) * 512], AF.Square, ["scr0"], ["m_junk", "g_ss"],
                            accum=ss[:, 3 + g:4 + g])
                    ACT(ss[:, 5:7], ss[:, 3:5], AF.Ln, ["g_ss"], ["g_ss"], bias=EPS, scale=1.0 / 512)
                    ACT(ss[:, 5:7], ss[:, 5:7], AF.Exp, ["g_ss"], ["g_ss"], scale=-0.5)
                    for g in range(2):
                        TS(hn[:, 1024 + g * 512:1024 + (g + 1) * 512], scr[0][:, g * 512:(g + 1) * 512], ss[:, 5 + g:6 + g],
                           None, ALU.mult, None, ["scr0", "g_ss"], ["hn"])
                TT(sm[:, 2, :], dt, Lend, ALU.mult, ["sm0", "dec"], ["sm2"])
                TT(xdtw[:].rearrange("p (h d) -> p h d", h=16), xs3, sm[:, 2, :].unsqueeze(2).to_broadcast([128, 16, 64]),
                   ALU.mult, ["xs_f", "sm2"], ["xdtw"])
                TT(S_f[:].rearrange("p (h d) -> p h d", h=16), S_f[:].rearrange("p (h d) -> p h d", h=16),
                   cdec.unsqueeze(2).to_broadcast([128, 16, 64]), ALU.mult, ["S_f", "dec"], ["S_f"])
                for g in range(2):
                    b = mmc["n"] % 2
                    mmc["n"] += 1
                    MM(pAB[b][:], bc_b[:, g * 128:(g + 1) * 128], xdtw[:, g * 512:(g + 1) * 512], True, True,
                       ["bc_b", "xdtw"], ["pAB%d" % b])
                    TT(S_f[:, g * 512:(g + 1) * 512], S_f[:, g * 512:(g + 1) * 512], pAB[b][:], ALU.add,
                       ["S_f", "pAB%d" % b], ["S_f"])
                CP(S_b[:], S_f[:], ["S_f"], ["S_b"], eng="act")
                drain()

            def attention(n, cur, prev):
                for half in range(2):
                    for hh in range(8):
                        h = half * 8 + hh
                        TR(pT[0:64, hh * 128:(hh + 1) * 128], q_sb[:, h * 64:(h + 1) * 64], idb[:], ["q_sb", "idb"], ["pT"])
                    CP(QT[0:64, half * 8:(half + 1) * 8, :], pT[0:64, :].rearrange("p (a t) -> p a t", a=8), ["pT"], ["QT"],
                       eng=("act" if half else "dve"))
                kv_to_attn(cur)
                slots = (0, prev, cur)
                for kv in range(4):
                    for seg in range(3):
                        if n == 0 and seg < 2:
                            src = bias_c0[seg, kv]
                        else:
                            src = bias_gen[seg, kv]
                        P.dma("sync", biasb[seg][:], src, writes=["biasb%d" % seg])
                        MM(pBig[:, seg * 512:(seg + 1) * 512], KT[slots[seg]][0:64, kv, :],
                           QT[0:64, 4 * kv:4 * kv + 4, :].rearrange("p a t -> p (a t)"), True, True,
                           ["KT%d" % slots[seg], "QT"], ["pBig%d" % seg])
                        STT(lgt[seg][:], pBig[:, seg * 512:(seg + 1) * 512], 0.125, biasb[seg][:], ALU.mult, ALU.add,
                            ["pBig%d" % seg, "biasb%d" % seg], ["lgt%d" % seg])
                        ACT(PT[seg][:].rearrange("p a t -> p (a t)"), lgt[seg][:], AF.Exp, ["lgt%d" % seg], ["PT%d" % seg])
                    for hl in range(4):
                        for seg in range(3):
                            MM(pBig[:, 1536 + hl * 65:1536 + (hl + 1) * 65], PT[seg][:, hl, :], vext[slots[seg]][:, kv, :],
                               seg == 0, seg == 2, ["PT%d" % seg, "vext%d" % slots[seg]], ["pBig3"])
                    CP(ao[:, 4 * kv:4 * kv + 4, :].rearrange("p a d -> p (a d)"), pBig[:, 1536:1536 + 260], ["pBig3"], ["ao"],
                       eng="act")
                TT(sm[:, 3, :], ao[:, :, 64], esink[:], ALU.add, ["ao", "esink"], ["sm3"])
                RECIP(sm[:, 3, :], sm[:, 3, :], ["sm3"], ["sm3"])
                TT(scr[1][:].rearrange("p (h d) -> p h d", h=16), ao[:, :, 0:64],
                   sm[:, 3, :].unsqueeze(2).to_broadcast([128, 16, 64]), ALU.mult, ["ao", "sm3"], ["scr1"])
                rms_rstd(scr[1][:], ss, junk[:, 0:1024], 1024, ["scr1"], "m")
                TS(hn[:, 0:1024], scr[1][:], ss[:, 2:3], None, ALU.mult, None, ["scr1", "m_ss"], ["hn"])

            def out_proj(n):
                transpose16(hn, 1, "hn")
                for t in range(4):
                    wi = load_w(8 + t)
                    pt, pk = proj_tile(wi)
                    xb = cstate["xb"]
                    TT(scr[t % 2][:, 0:512], pt[:], x_t[xb][:, t * 512:(t + 1) * 512], ALU.add, [pk, "x_t%d" % xb],
                       ["scr%d" % (t % 2)])
                    P.dma("sync", h2s[n * 128:(n + 1) * 128, t * 512:(t + 1) * 512], scr[t % 2][:, 0:512],
                          reads=["scr%d" % (t % 2)], writes=["h2s%d" % n])

            for _ in range(NW):
                issue_w()
            issue_x(0)
            issue_x(1)
            norm_T(0)
            wi = load_w(2)
            pt, pk = proj_tile(wi)
            CP(kv_sb[:], pt[:], [pk], ["kv_sb"], eng="act")
            kv_to_attn(0)
            par = 0
            for ci in range(NPRE):
                chunk(ci, "pre_kv" if ci == NPRE - 1 else "pre", par)
                par = 1 - par
            kv_to_attn(1)
            prev = 1
            for n in range(NMAIN):
                cur = 3 - prev
                chunk(NPRE + n, "main", par)
                par = 1 - par
                attention(n, cur, prev)
                out_proj(n)
                prev = cur
            P.barrier()
            P.emit()

        with ExitStack() as st2:
            sb = lambda name, shape, dt: st2.enter_context(nc.sbuf_tensor(name, shape, dt))
            pa0 = st2.enter_context(nc.psum_tensor("pa0", [128, 512], F32))
            pacc = st2.enter_context(nc.psum_tensor("pacc", [128, 2048], F32))
            lnf = sb("lnf", [128, D], F32)
            acc = sb("acc", [128, 4, D], F32)
            xn2T = sb("xn2T", [128, 16, 512], BF16)
            ss2 = sb("ss2", [128, 8], F32)
            A1 = [sb("A1_%d" % t, [128, 8, 128], F32) for t in range(4)]
            S2 = [sb("S2_%d" % t, [128, 8, 128], F32) for t in range(4)]
            Dk = [sb("Dk_%d" % t, [128, 8, 128], BF16) for t in range(4)]
            NU = 3
            NV = 8

            P.dma("sync", lnf[:], lnfin, writes=["lnf"])

            for grp in range(2):
                with ExitStack() as st3:
                    pT2 = st3.enter_context(nc.psum_tensor("pT2_%d" % grp, [128, 1024], BF16))
                    pa1 = st3.enter_context(nc.psum_tensor("pa1_%d" % grp, [128, 512], F32))
                    pa = [pa0, pa1]
                    sb3 = lambda name, shape, dt: st3.enter_context(nc.sbuf_tensor(name + "_g%d" % grp, shape, dt))
                    keysb = sb3("keysb", [128, 16, 128], BF16)
                    junk2 = sb3("junk2", [128, D], BF16)
                    P.dma("pool", keysb[:], keysT, writes=["keysb"])
                    qTc = [sb3("qTc%d" % i, [128, 512], BF16) for i in range(2)]
                    scall = sb3("scall", [128, 4, 16, 128], F32)
                    topsall = sb3("topsall", [128, 4, 16, 16], F32)
                    xn2 = sb3("xn2", [128, D], BF16)
                    scw = [sb3("scw%d" % i, [128, 4, 128], F32) for i in range(2)]
                    cand = sb3("cand", [128, 8, 256], F32)
                    candw = sb3("candw", [128, 8, 256], F32)
                    ctop = sb3("ctop", [128, 8, 16], F32)
                    st8 = sb3("st8", [128, 6, 8], F32)
                    wqb = [sb3("wqb%d" % i, [128, 16, 128], BF16) for i in range(2)]
                    for tt in range(4):
                        n = grp * 4 + tt
                        P.dma("sync", acc[:, tt, :], h2s[n * 128:(n + 1) * 128, :], reads=["h2s%d" % n], writes=["acc%d" % tt])
                        rms_rstd(acc[:, tt, :], ss2, junk2[:], D, ["acc%d" % tt], "p")
                        TS(xn2[:], acc[:, tt, :], ss2[:, 2:3], None, ALU.mult, None, ["acc%d" % tt, "p_ss"], ["xn2"])
                        for half in range(2):
                            for kk in range(8):
                                k = half * 8 + kk
                                TR(pT2[:, kk * 128:(kk + 1) * 128], xn2[:, k * 128:(k + 1) * 128], idb[:], ["xn2", "idb"], ["pT2"])
                            for kk in range(8):
                                k = half * 8 + kk
                                TS(xn2T[:, k, tt * 128:(tt + 1) * 128], pT2[:, kk * 128:(kk + 1) * 128], nwt[:, 2, k:k + 1],
                                   None, ALU.mult, None, ["pT2", "nwt"], ["xn2T"])
                    for c in range(16):
                        wi = c % 2
                        P.dma("pool", wqb[wi][:], wq_blk[c], writes=["wqb%d" % wi])
                        for k in range(16):
                            MM(pa[wi][:], wqb[wi][:, k, :], xn2T[:, k, :], k == 0, k == 15, ["wqb%d" % wi, "xn2T"], ["pa%d" % wi])
                        CP(qTc[wi][:], pa[wi][:], ["pa%d" % wi], ["qTc%d" % wi], eng="act")
                        pb = c % 4
                        for tt in range(4):
                            MM(pacc[:, pb * 512 + tt * 128:pb * 512 + (tt + 1) * 128], qTc[wi][:, tt * 128:(tt + 1) * 128],
                               keysb[:, c, :], True, True, ["qTc%d" % wi, "keysb"], ["pacc%d" % pb])
                        CP(scall[:, :, c, :], pacc[:, pb * 512:(pb + 1) * 512].rearrange("p (a k) -> p a k", a=4),
                           ["pacc%d" % pb], ["sc_%d" % c], eng="act")
                        sw = c % 2
                        for tt in range(4):
                            OP("dve", lambda e, c=c, tt=tt: e.max(out=topsall[:, tt, c, 0:8], in_=scall[:, tt, c, :]),
                               ["sc_%d" % c], ["tA%d_%d" % (tt, c)])
                        for tt in range(4):
                            OP("dve", lambda e, c=c, tt=tt, sw=sw: e.match_replace(out=scw[sw][:, tt, :],
                                                                                   in_to_replace=topsall[:, tt, c, 0:8],
                                                                                   in_values=scall[:, tt, c, :], imm_value=NEG),
                               ["sc_%d" % c, "tA%d_%d" % (tt, c)], ["scw%d_%d" % (sw, tt)])
                        for tt in range(4):
                            OP("dve", lambda e, c=c, tt=tt, sw=sw: e.max(out=topsall[:, tt, c, 8:16], in_=scw[sw][:, tt, :]),
                               ["scw%d_%d" % (sw, tt)], ["tB%d_%d" % (tt, c)])
                    sck = ["sc_%d" % c for c in range(16)]
                    for tt in range(4):
                        tops = topsall[:, tt]
                        sc = scall[:, tt]
                        tk = ["tA%d_%d" % (tt, c) for c in range(16)] + ["tB%d_%d" % (tt, c) for c in range(16)]
                        t4 = tops.rearrange("p (h c) k -> p h c k", c=2)
                        TT(cand[:].rearrange("p h (a b) -> p h a b", a=16),
                           t4[:, :, 0, :].unsqueeze(3).to_broadcast([128, 8, 16, 16]),
                           t4[:, :, 1, :].unsqueeze(2).to_broadcast([128, 8, 16, 16]), ALU.add, tk, ["cand"])
                        for h in range(8):
                            OP("dve", lambda e, h=h: e.max(out=ctop[:, h, 0:8], in_=cand[:, h, :]), ["cand"], ["ctopA%d" % h])
                        for h in range(8):
                            OP("dve", lambda e, h=h: e.match_replace(out=candw[:, h, :], in_to_replace=ctop[:, h, 0:8],
                                                                       in_values=cand[:, h, :], imm_value=NEG),
                               ["cand", "ctopA%d" % h], ["candw%d" % h])
                        for h in range(8):
                            OP("dve", lambda e, h=h: e.max(out=ctop[:, h, 8:16], in_=candw[:, h, :]), ["candw%d" % h], ["ctopB%d" % h])
                        ck = ["ctopA%d" % h for h in range(8)] + ["ctopB%d" % h for h in range(8)]
                        TT(cand[:, :, 0:16], ctop[:], ctop[:, :, 0:1].to_broadcast([128, 8, 16]), ALU.subtract, ck, ["cand"])
                        ACT(cand[:, :, 0:16], cand[:, :, 0:16], AF.Exp, ["cand"], ["cand"])
                        OP("dve", lambda e: e.tensor_reduce(out=st8[:, 0, :], in_=cand[:, :, 0:16], axis=AX.X, op=ALU.add),
                           ["cand"], ["st8"])
                        RECIP(st8[:, 1, :], st8[:, 0, :], ["st8"], ["st8"])
                        TT(st8[:, 2, :], cand[:, :, 15], st8[:, 1, :], ALU.mult, ["cand", "st8"], ["st8"])
                        TS(st8[:, 3, :], ctop[:, :, 15], -DELTA, None, ALU.add, None, ck, ["st8"])
                        sc4 = sc.rearrange("p (h c) k -> p h c k", c=2)
                        TT(A1[tt][:], sc4[:, :, 0, :], st8[:, 3, :].unsqueeze(2).to_broadcast([128, 8, 128]), ALU.subtract,
                           sck + ["st8"], ["A1_%d" % tt])
                        CP(S2[tt][:], sc4[:, :, 1, :], sck, ["S2_%d" % tt])
                        TT(Dk[tt][:], ident_f.unsqueeze(1).to_broadcast([128, 8, 128]),
                           st8[:, 2, :].unsqueeze(2).to_broadcast([128, 8, 128]), ALU.mult, ["cmt", "st8"], ["Dk_%d" % tt])
                    P.barrier()
                    P.emit()
                with ExitStack() as st3:
                    pG = st3.enter_context(nc.psum_tensor("pG_%d" % grp, [128, 512], F32))
                    pGT = st3.enter_context(nc.psum_tensor("pGT_%d" % grp, [128, 2048], BF16))
                    sb3 = lambda name, shape, dt: st3.enter_context(nc.sbuf_tensor(name + "_g%d" % grp, shape, dt))
                    ub = [sb3("ub%d" % i, [128, 16, 128], BF16) for i in range(NU)]
                    vb = [sb3("vb%d" % i, [128, D], BF16) for i in range(NV)]
                    aTs = sb3("aTs", [128, IA, 512], F32)
                    gaT = [sb3("gaT%d" % i, [128, IA, 512], BF16) for i in range(2)]
                    NR = 3
                    cB = [sb3("cB%d" % i, [128, 8, 128], F32) for i in range(NR)]
                    Eb = [sb3("Eb%d" % i, [128, 8, 128], F32) for i in range(2)]
                    MEq = [sb3("MEq%d" % i, [128, IA, 8, 128], BF16) for i in range(2)]
                    Gs = [sb3("Gs%d" % i, [128, IA, 128], BF16) for i in range(2)]
                    wTs = [sb3("wTs%d" % i, [128, 512], BF16) for i in range(2 * IA)]
                    NBLK = 128 // IA
                    st = {"un": 0, "vn": 0, "en": 0, "ee": 0, "mq": 0, "gs": 0}

                    def u_mm(blk, il):
                        i = blk * IA + il
                        us = st["un"] % NU
                        st["un"] += 1
                        P.dma("pool", ub[us][:], u_blk[i], writes=["ub%d" % us])
                        for k in range(16):
                            MM(pa0[:], ub[us][:, k, :], xn2T[:, k, :], k == 0, k == 15, ["ub%d" % us, "xn2T"], ["pa0"])

                    def u_copy(il):
                        CP(aTs[:, il, :], pa0[:], ["pa0"], ["aTs%d" % il], eng="dve")

                    def gelu_stage(blk):
                        gq = blk % 2
                        ACT(gaT[gq][:].rearrange("p a t -> p (a t)"), aTs[:].rearrange("p a t -> p (a t)"), AF.Gelu,
                            ["aTs%d" % il for il in range(IA)], ["gaT%d" % gq])

                    def v_mm(blk, tt, vsl):
                        gq = blk % 2
                        for hq in range(2):
                            for il in range(IA):
                                ws = gq * IA + il
                                for dd in range(2):
                                    dq = hq * 2 + dd
                                    MM(pacc[:, dq * 512:(dq + 1) * 512], wTs[ws][:, tt * 128:(tt + 1) * 128],
                                       vb[vsl[il]][:, dq * 512:(dq + 1) * 512], il == 0, il == IA - 1,
                                       ["wTs%d" % ws, "vb%d" % vsl[il]], ["pacc%d" % dq])

                    def v_add(tt):
                        for hq in range(2):
                            TT(acc[:, tt, hq * 1024:(hq + 1) * 1024], acc[:, tt, hq * 1024:(hq + 1) * 1024],
                               pacc[:, hq * 1024:(hq + 1) * 1024], ALU.add,
                               ["acc%d_%d" % (tt, hq), "pacc%d" % (2 * hq), "pacc%d" % (2 * hq + 1)], ["acc%d_%d" % (tt, hq)])

                    def wT_stage(blk):
                        gq = blk % 2
                        for il in range(IA):
                            ws = gq * IA + il
                            TT(wTs[ws][:], gaT[gq][:, il, :], pGT[:, il * 512:(il + 1) * 512], ALU.mult,
                               ["gaT%d" % gq, "pGT%d" % il], ["wTs%d" % ws])

                    def gt_stage(pg):
                        tt_, gs_ = pg
                        CP(Gs[gs_][:].rearrange("p a j -> p (a j)"), pG[:], ["pG"], ["Gs%d" % gs_], eng="dve")
                        for il in range(IA):
                            TR(pGT[:, (il * 4 + tt_) * 128:(il * 4 + tt_ + 1) * 128], Gs[gs_][:, il, :], idb[:],
                               ["Gs%d" % gs_, "idb"], ["pGT%d" % il])

                    for il in range(IA):
                        u_mm(0, il)
                        u_copy(il)
                    gelu_stage(0)
                    prev_vsl = None
                    pend_add = None
                    pend_wT = None
                    pend_G = None
                    pend_gelu = None
                    for blk in range(NBLK):
                        vsl = []
                        for il in range(IA):
                            vs = st["vn"] % NV
                            st["vn"] += 1
                            vsl.append(vs)
                        for tt in range(4):
                            if blk + 1 < NBLK:
                                u_mm(blk + 1, tt)
                            mq = st["mq"] % 2
                            st["mq"] += 1
                            for il in range(IA):
                                i = blk * IA + il
                                eb = st["en"] % NR
                                st["en"] += 1
                                ee = st["ee"] % 2
                                st["ee"] += 1
                                TT(cB[eb][:], S2[tt][:], A1[tt][:, :, i:i + 1].to_broadcast([128, 8, 128]), ALU.add,
                                   ["S2_%d" % tt, "A1_%d" % tt], ["cB%d" % eb], eng="pool")
                                ACT(Eb[ee][:].rearrange("p h j -> p (h j)"), cB[eb][:].rearrange("p h j -> p (h j)"), AF.Prelu,
                                    ["cB%d" % eb], ["Eb%d" % ee], alpha=1e10)
                                ACT(MEq[mq][:, il, :, :].rearrange("p h j -> p (h j)"), Eb[ee][:].rearrange("p h j -> p (h j)"), AF.Exp,
                                    ["Eb%d" % ee], ["MEq%d" % mq])
                            P.dma("pool", vb[vsl[tt]][:], v_blk[blk * IA + tt], writes=["vb%d" % vsl[tt]])
                            if pend_G is not None:
                                gt_stage(pend_G)
                                pend_G = None
                            if pend_add is not None:
                                v_add(pend_add)
                                pend_add = None
                            if pend_wT is not None:
                                wT_stage(pend_wT)
                                pend_wT = None
                            if pend_gelu is not None:
                                gelu_stage(pend_gelu)
                                pend_gelu = None
                            if blk + 1 < NBLK:
                                u_copy(tt)
                            for h in range(8):
                                MM(pG[:], Dk[tt][:, h, :], MEq[mq][:, :, h, :], h == 0, h == 7,
                                   ["Dk_%d" % tt, "MEq%d" % mq], ["pG"])
                            gs = st["gs"] % 2
                            st["gs"] += 1
                            pend_G = (tt, gs)
                            if blk > 0:
                                v_mm(blk - 1, tt, prev_vsl)
                                pend_add = tt
                        pend_wT = blk
                        if blk + 1 < NBLK:
                            pend_gelu = blk + 1
                        prev_vsl = vsl
                    if pend_add is not None:
                        v_add(pend_add)
                    gt_stage(pend_G)
                    wT_stage(NBLK - 1)
                    for tt in range(4):
                        v_mm(NBLK - 1, tt, prev_vsl)
                        v_add(tt)
                    jk = MEq[0][:].rearrange("p a h j -> p (a h j)")[:, 0:D]
                    for tt in range(4):
                        n = grp * 4 + tt
                        ak = ["acc%d_0" % tt, "acc%d_1" % tt]
                        rms_rstd(acc[:, tt, :], ss2, jk, D, ak, "p", jkey="MEq0")
                        STT(acc[:, tt, :], acc[:, tt, :], ss2[:, 2:3], lnf[:], ALU.mult, ALU.mult, ak + ["p_ss", "lnf"], ak)
                        P.dma("sync", out[n * 128:(n + 1) * 128, :], acc[:, tt, :], reads=ak, writes=["out%d" % n])
                    P.barrier()
                    P.emit()
    return nc


def _t5_bucket(dist):
    n = np.maximum(dist, 0)
    max_exact = 16
    large = max_exact + (np.log(np.maximum(n, 1) / max_exact) / np.log(128 / max_exact) * (32 - max_exact)).astype(np.int32)
    large = np.minimum(large, 31)
    return np.where(n < max_exact, n, large).astype(np.int32)


def _bias_tables(rel_bias, blk):
    qi = np.arange(128)[None, :]
    kj = np.arange(128)[:, None]
    out = np.full((3, 16, 128, 128), NEG, np.float32)
    rb = rel_bias.astype(np.float32)
    m = np.arange(16)[:, None]
    dist = 16 + blk * 128 + qi - m
    out[0, :, 112:128, :] = np.transpose(rb[_t5_bucket(dist)], (2, 0, 1))
    if blk > 0:
        dist = 128 + qi - kj
        valid = (dist < 128)
        g = np.transpose(rb[_t5_bucket(dist)], (2, 0, 1))
        out[1] = np.where(valid[None], g, NEG)
    dist = qi - kj
    valid = dist >= 0
    g = np.transpose(rb[_t5_bucket(dist)], (2, 0, 1))
    out[2] = np.where(valid[None], g, NEG)
    o = out.reshape(3, 4, 4, 128, 128).transpose(0, 1, 3, 2, 4).reshape(3, 4, 128, 512)
    return np.ascontiguousarray(o)


_NC_CACHE = {}


def kernel(x, meta_tokens, rel_bias, ln_mix, w_in, attn_sinks, conv_w, conv_b, dt_bias, a_log, d_skip,
           attn_norm_w, ssm_norm_w, w_out, ln_ffn, peer_wq, peer_keys, peer_u, peer_v, ln_final):
    f = lambda a: np.asarray(a, dtype=np.float32)
    x = f(x); meta = f(meta_tokens); rel_bias = f(rel_bias)
    w_in = f(w_in)[0]; w_out = f(w_out)[0]; wq = f(peer_wq)[0]
    keys = f(peer_keys)[0]; u = f(peer_u)[0]; v = f(peer_v)[0]
    rep = lambda a, shape: np.ascontiguousarray(np.broadcast_to(a, shape)).astype(np.float32)
    blkw = lambda w, nt, cw: np.ascontiguousarray(w.reshape(16, 128, nt, cw).transpose(2, 1, 0, 3))
    w_in_blk = blkw(w_in[:, :4096], 8, 512)
    w_dt = np.ascontiguousarray(w_in[:, 4096:4112].reshape(16, 128, 16).transpose(1, 0, 2))
    w_out_blk = blkw(w_out, 4, 512)
    wq_blk = blkw(wq, 16, 128)
    keysT = np.ascontiguousarray(keys.reshape(16, 128, 128).transpose(2, 0, 1))
    u_blk = np.ascontiguousarray(u.reshape(128, 128, 16, 128).transpose(0, 3, 2, 1))
    v_blk = v.reshape(128, 128, D)
    convw_r = rep(f(conv_w)[0][None], (128, 4, 1536))
    convb_r = f(conv_b)[0][None, :]
    hv = rep(np.stack([f(dt_bias)[0], f(a_log)[0], f(d_skip)[0], f(attn_sinks)[0]])[None], (128, 4, 16))
    tl = lambda w: w.reshape(16, 128).T
    nwa = np.ascontiguousarray(np.stack([tl(f(ln_mix)[0]), tl(np.concatenate([f(attn_norm_w)[0], f(ssm_norm_w)[0]])),
                                         tl(f(ln_ffn)[0])], axis=1)).astype(np.float32)
    lnfin = rep(f(ln_final)[None], (128, D))
    ar = np.arange(128)
    cmat = np.stack([np.eye(128), (ar[:, None] <= ar[None, :]), (ar[:, None] > ar[None, :]), np.ones((128, 128))],
                    axis=1).astype(np.float32)
    sh = np.zeros((128, 7, 128), np.float32)
    for j in range(4):
        s = 3 - j
        sh[:, j, :] = (ar[:, None] == ar[None, :] - s)
    for j in range(3):
        s = 3 - j
        sh[:, 4 + j, :] = (ar[:, None] == 128 + ar[None, :] - s)
    bias_gen = _bias_tables(rel_bias, 1)
    bias_first = _bias_tables(rel_bias, 0)
    common = dict(w_in_blk=w_in_blk, w_dt=w_dt, w_out_blk=w_out_blk, wq_blk=wq_blk, keysT=keysT, u_blk=u_blk,
                  v_blk=v_blk, convw_r=convw_r, convb_r=convb_r, hv=hv, nw=nwa, lnfin=lnfin, cm=cmat, shm_r=sh,
                  bias_gen=bias_gen)
    xmeta = np.zeros((128, D), np.float32)
    xmeta[112:] = meta
    in_maps = []
    for c in range(NCORES):
        b, half = c // 2, c % 2
        xt = np.zeros(((NPRE + NMAIN) * 128, D), np.float32)
        pm = np.zeros((128, NPRE), np.float32)
        if half == 1:
            xt[112:128] = meta
            xt[128:] = x[b]
            pm[112:, 0] = 1.0
            pm[:, 1:] = 1.0
            bc0 = bias_gen[0:2]
        else:
            xt[NPRE * 128 - 16:NPRE * 128] = meta
            xt[NPRE * 128:] = x[b, :1024]
            pm[112:, NPRE - 1] = 1.0
            bc0 = bias_first[0:2]
        d = dict(common)
        d.update(xtok=xt, xmeta=xmeta, padmask=pm, bias_c0=np.ascontiguousarray(bc0))
        in_maps.append(d)
    if "nc" not in _NC_CACHE:
        _NC_CACHE["nc"] = build_nc()
    res = run_bass_kernel_spmd(_NC_CACHE["nc"], in_maps, core_ids=list(range(NCORES)))
    outp = np.empty((4, 2048, D), np.float32)
    for c in range(NCORES):
        b, half = c // 2, c % 2
        outp[b, half * 1024:(half + 1) * 1024] = res.results[c]["out"]
    return outp
```
